# Optimizing a Trainium2 kernel written in Bass

```python
import jax, jax.numpy as jnp
from jax import lax
import numpy as np

D_MODEL = 2048
BATCH = 8
SEQ = 4096
DEPTH = 4

N_HEADS = 16
HEAD_DIM = D_MODEL // N_HEADS
ATTN_GROUPS = ((128, 1), (512, 4), (2048, 16))
N_GROUPS = len(ATTN_GROUPS)
Q_BLOCK = 128
D_RNN = D_MODEL
RNN_BLOCK = 256
N_RNN_BLOCKS = D_RNN // RNN_BLOCK
CONV_WIDTH = 4
LRU_C = 8.0
D_FF = ((8 * D_MODEL // 3 + 511) // 512) * 512
N_EXPERTS = 8
TOP_K = 2
EXPERT_BLOCK = 256
ADA_SCALE = 0.1
LN_EPS = 1e-5
NEG_INF = -1e30
DEEPNORM_ALPHA = (2 * DEPTH) ** 0.25
DEEPNORM_BETA = (8 * DEPTH) ** -0.25
N_ATTN_LAYERS = (DEPTH + 1) // 2
N_RNN_LAYERS = DEPTH // 2

kernel_name = 'hybrid_dilated_attn_rglru_moe_trunk'


def alibi_slopes():
    return 2.0 ** (-8.0 * jnp.arange(1, N_HEADS + 1, dtype=jnp.float32) / N_HEADS)


def layer_norm(x, g, b):
    xf = x.astype(jnp.float32)
    mu = xf.mean(-1, keepdims=True)
    var = jnp.square(xf - mu).mean(-1, keepdims=True)
    y = (xf - mu) * lax.rsqrt(var + LN_EPS) * g.astype(jnp.float32) + b.astype(jnp.float32)
    return y.astype(x.dtype)


def dilated_window_attention(q, k, v, steps, dilation, slopes):
    B, S, H, Dh = q.shape
    L = S // dilation
    nb = -(-L // Q_BLOCK)
    Lp = nb * Q_BLOCK

    def to_blocks(t):
        t = t.reshape(B, L, dilation, H, Dh).transpose(0, 2, 1, 3, 4)
        t = jnp.pad(t, ((0, 0), (0, 0), (0, Lp - L), (0, 0), (0, 0)))
        return t.reshape(B, dilation, nb, Q_BLOCK, H, Dh).astype(jnp.float32)

    def with_prev(t):
        prev = jnp.pad(t, ((0, 0), (0, 0), (1, 0), (0, 0), (0, 0), (0, 0)))[:, :, :-1]
        return jnp.concatenate([prev, t], axis=3)

    qb = to_blocks(q)
    kk = with_prev(to_blocks(k))
    vv = with_prev(to_blocks(v))
    s = jnp.einsum('brnqhe,brnkhe->brnhqk', qb, kk) * (Dh ** -0.5)
    qi = jnp.arange(Q_BLOCK)[:, None]
    ki = jnp.arange(2 * Q_BLOCK)[None, :]
    rel = qi + Q_BLOCK - ki
    key_pos = jnp.arange(nb)[:, None, None] * Q_BLOCK - Q_BLOCK + ki[None]
    valid = (rel >= 0)[None] & (rel <= steps)[None] & (key_pos >= 0)
    alibi = -slopes[:, None, None] * (rel * dilation).astype(jnp.float32)[None]
    s = jnp.where(valid[:, None], s + alibi, NEG_INF)
    m = s.max(-1)
    p = jnp.exp(s - m[..., None])
    l = p.sum(-1)
    o = jnp.einsum('brnhqk,brnkhe->brnqhe', p, vv) / jnp.swapaxes(l, -1, -2)[..., None]
    lse = jnp.swapaxes(m + jnp.log(l), -1, -2)
    o = o.reshape(B, dilation, Lp, H, Dh)[:, :, :L].transpose(0, 2, 1, 3, 4).reshape(B, S, H, Dh)
    lse = lse.reshape(B, dilation, Lp, H)[:, :, :L].transpose(0, 2, 1, 3).reshape(B, S, H)
    return o, lse


def attention_mixer(u, w_qkv, w_o):
    B, S, _ = u.shape
    qkv = (u @ w_qkv).reshape(B, S, N_GROUPS, 3, N_HEADS, HEAD_DIM)
    slopes = alibi_slopes()
    outs, lses = [], []
    for g, (window, dilation) in enumerate(ATTN_GROUPS):
        o, lse = dilated_window_attention(qkv[:, :, g, 0], qkv[:, :, g, 1], qkv[:, :, g, 2],
                                          window // dilation, dilation, slopes)
        outs.append(o)
        lses.append(lse)
    wts = jax.nn.softmax(jnp.stack(lses, axis=0), axis=0)
    o = outs[0] * wts[0][..., None]
    for g in range(1, N_GROUPS):
        o = o + outs[g] * wts[g][..., None]
    return o.reshape(B, S, N_HEADS * HEAD_DIM).astype(u.dtype) @ w_o


def _lru_combine(left, right):
    a_l, b_l = left
    a_r, b_r = right
    return a_l * a_r, a_r * b_l + b_r


def rglru_mixer(u, w_in, conv_w, conv_b, ga_w, ga_b, gx_w, gx_b, lam, w_out):
    B, S, _ = u.shape
    gate_br, rec = jnp.split(u @ w_in, 2, axis=-1)
    xc = lax.conv_general_dilated(rec, conv_w[:, None, :], window_strides=(1,),
                                  padding=((CONV_WIDTH - 1, 0),),
                                  dimension_numbers=('NWC', 'WIO', 'NWC'),
                                  feature_group_count=D_RNN) + conv_b
    xblk = xc.reshape(B, S, N_RNN_BLOCKS, RNN_BLOCK)
    r = jax.nn.sigmoid(jnp.einsum('bsni,nio->bsno', xblk, ga_w).reshape(B, S, D_RNN).astype(jnp.float32)
                       + ga_b.astype(jnp.float32))
    i = jax.nn.sigmoid(jnp.einsum('bsni,nio->bsno', xblk, gx_w).reshape(B, S, D_RNN).astype(jnp.float32)
                       + gx_b.astype(jnp.float32))
    log_a = -LRU_C * r * jax.nn.softplus(-lam.astype(jnp.float32))
    a = jnp.exp(log_a)
    b = jnp.sqrt(-jnp.expm1(2.0 * log_a)) * (i * xc.astype(jnp.float32))
    _, h = lax.associative_scan(_lru_combine, (a, b), axis=1)
    y = (jax.nn.gelu(gate_br.astype(jnp.float32)) * h).astype(u.dtype)
    return y @ w_out


def dense_swiglu(u, w_in, w_out):
    g, up = jnp.split(u @ w_in, 2, axis=-1)
    return (jax.nn.silu(g) * up) @ w_out


def moe_swiglu(u, w_router, w_in, w_out):
    B, S, D = u.shape
    T = B * S
    A = T * TOP_K
    xt = u.reshape(T, D)
    logits = (xt @ w_router).astype(jnp.float32)
    top_logits, top_idx = lax.top_k(logits, TOP_K)
    top_w = jax.nn.softmax(top_logits, axis=-1)
    e_flat = top_idx.reshape(-1)
    w_flat = top_w.reshape(-1)
    tok_flat = jnp.arange(A, dtype=jnp.int32) // TOP_K
    order = jnp.argsort(e_flat)
    e_s, tok_s, w_s = e_flat[order], tok_flat[order], w_flat[order]
    counts = jnp.bincount(e_flat, length=N_EXPERTS)
    padded = (counts + EXPERT_BLOCK - 1) // EXPERT_BLOCK * EXPERT_BLOCK
    pad_end = jnp.cumsum(padded)
    pad_start = pad_end - padded
    cnt_start = jnp.cumsum(counts) - counts
    dest = pad_start[e_s] + (jnp.arange(A) - cnt_start[e_s])
    P = A + N_EXPERTS * EXPERT_BLOCK
    n_blk = P // EXPERT_BLOCK
    buf_tok = jnp.full((P,), T, dtype=jnp.int32).at[dest].set(tok_s)
    buf_w = jnp.zeros((P,), jnp.float32).at[dest].set(w_s)
    blk_expert = jnp.minimum(jnp.searchsorted(pad_end, jnp.arange(n_blk) * EXPERT_BLOCK, side='right'),
                             N_EXPERTS - 1)
    x_pad = jnp.concatenate([xt, jnp.zeros((1, D), xt.dtype)], axis=0)
    xb = x_pad[buf_tok].reshape(n_blk, EXPERT_BLOCK, D)

    def expert_block(args):
        xblk, e = args
        g, up = jnp.split(xblk @ w_in[e], 2, axis=-1)
        return (jax.nn.silu(g) * up) @ w_out[e]

    yb = lax.map(expert_block, (xb, blk_expert)).reshape(P, D)
    y = jnp.zeros((T + 1, D), jnp.float32).at[buf_tok].add(yb.astype(jnp.float32) * buf_w[:, None])[:T]
    return y.astype(u.dtype).reshape(B, S, D)


def setup_inputs(seed: int = 0) -> dict:
    key = jax.random.key(seed)
    ks = jax.random.split(key, 22)
    D = D_MODEL
    nrm = jax.random.normal
    beta = DEEPNORM_BETA
    qkv_scale = jnp.array([1.0, 1.0, beta], dtype=jnp.float32).reshape(1, 1, 1, 3, 1)
    a0 = jax.random.uniform(ks[15], (N_RNN_LAYERS, D_RNN), minval=0.9, maxval=0.999)
    return {
        'x': nrm(ks[0], (BATCH, SEQ, D), jnp.float32),
        'c': nrm(ks[1], (BATCH, D), jnp.float32),
        'ada_w': nrm(ks[2], (DEPTH, D, 6 * D), jnp.float32) * (ADA_SCALE * D ** -0.5),
        'ada_b': 0.01 * nrm(ks[3], (DEPTH, 6 * D), jnp.float32),
        'ln_g': 1.0 + 0.02 * nrm(ks[4], (DEPTH, 2, D), jnp.float32),
        'ln_b': 0.02 * nrm(ks[5], (DEPTH, 2, D), jnp.float32),
        'attn_w_qkv': (nrm(ks[6], (N_ATTN_LAYERS, D, N_GROUPS, 3, N_HEADS * HEAD_DIM), jnp.float32)
                       * (D ** -0.5) * qkv_scale).reshape(N_ATTN_LAYERS, D, N_GROUPS * 3 * N_HEADS * HEAD_DIM),
        'attn_w_o': nrm(ks[7], (N_ATTN_LAYERS, N_HEADS * HEAD_DIM, D), jnp.float32) * ((N_HEADS * HEAD_DIM) ** -0.5 * beta),
        'rg_w_in': nrm(ks[8], (N_RNN_LAYERS, D, 2 * D_RNN), jnp.float32) * D ** -0.5,
        'rg_conv_w': nrm(ks[9], (N_RNN_LAYERS, CONV_WIDTH, D_RNN), jnp.float32) * CONV_WIDTH ** -0.5,
        'rg_conv_b': 0.01 * nrm(ks[10], (N_RNN_LAYERS, D_RNN), jnp.float32),
        'rg_gate_a_w': nrm(ks[11], (N_RNN_LAYERS, N_RNN_BLOCKS, RNN_BLOCK, RNN_BLOCK), jnp.float32) * RNN_BLOCK ** -0.5,
        'rg_gate_a_b': 0.01 * nrm(ks[12], (N_RNN_LAYERS, D_RNN), jnp.float32),
        'rg_gate_x_w': nrm(ks[13], (N_RNN_LAYERS, N_RNN_BLOCKS, RNN_BLOCK, RNN_BLOCK), jnp.float32) * RNN_BLOCK ** -0.5,
        'rg_gate_x_b': 0.01 * nrm(ks[14], (N_RNN_LAYERS, D_RNN), jnp.float32),
        'rg_lambda': jnp.log(a0) - jnp.log1p(-a0),
        'rg_w_out': nrm(ks[16], (N_RNN_LAYERS, D_RNN, D), jnp.float32) * (D_RNN ** -0.5 * beta),
        'ffn_w_in': nrm(ks[17], (N_ATTN_LAYERS, D, 2 * D_FF), jnp.float32) * D ** -0.5,
        'ffn_w_out': nrm(ks[18], (N_ATTN_LAYERS, D_FF, D), jnp.float32) * (D_FF ** -0.5 * beta),
        'moe_w_router': nrm(ks[19], (N_RNN_LAYERS, D, N_EXPERTS), jnp.float32) * D ** -0.5,
        'moe_w_in': nrm(ks[20], (N_RNN_LAYERS, N_EXPERTS, D, 2 * D_FF), jnp.float32) * D ** -0.5,
        'moe_w_out': nrm(ks[21], (N_RNN_LAYERS, N_EXPERTS, D_FF, D), jnp.float32) * (D_FF ** -0.5 * beta),
    }


def reference(x, c, ada_w, ada_b, ln_g, ln_b, attn_w_qkv, attn_w_o, rg_w_in, rg_conv_w, rg_conv_b,
              rg_gate_a_w, rg_gate_a_b, rg_gate_x_w, rg_gate_x_b, rg_lambda, rg_w_out,
              ffn_w_in, ffn_w_out, moe_w_router, moe_w_in, moe_w_out):
    c_act = jax.nn.silu(c)
    for i in range(DEPTH):
        j = i // 2
        mod = c_act @ ada_w[i] + ada_b[i]
        sh1, sc1, g1, sh2, sc2, g2 = jnp.split(mod[:, None, :], 6, axis=-1)
        u = x * (1.0 + sc1) + sh1
        if i % 2 == 0:
            y = attention_mixer(u, attn_w_qkv[j], attn_w_o[j])
        else:
            y = rglru_mixer(u, rg_w_in[j], rg_conv_w[j], rg_conv_b[j], rg_gate_a_w[j], rg_gate_a_b[j],
                            rg_gate_x_w[j], rg_gate_x_b[j], rg_lambda[j], rg_w_out[j])
        x = layer_norm(DEEPNORM_ALPHA * x + (1.0 + g1) * y, ln_g[i, 0], ln_b[i, 0])
        u = x * (1.0 + sc2) + sh2
        if i % 2 == 0:
            y = dense_swiglu(u, ffn_w_in[j], ffn_w_out[j])
        else:
            y = moe_swiglu(u, moe_w_router[j], moe_w_in[j], moe_w_out[j])
        x = layer_norm(DEEPNORM_ALPHA * x + (1.0 + g2) * y, ln_g[i, 1], ln_b[i, 1])
    return x
```

```python
from contextlib import ExitStack

import numpy as np

import concourse.bass as bass
import concourse.mybir as mybir
from concourse.bass_utils import run_bass_kernel_spmd

F32 = mybir.dt.float32
BF16 = mybir.dt.bfloat16
ALU = mybir.AluOpType
AF = mybir.ActivationFunctionType

D = 2048
SEQ = 4096
KC = 16
TT = 512
NT = SEQ // TT
DFF = 5632
JC = DFF // 128
NH = 16
NE = 8
DEPTH = 4
ALPHA = float((2 * DEPTH) ** 0.25)
LN_EPS = 1e-5
SCALE = float(128 ** -0.5)
BIGREL = 1.0e5
DILS = (1, 4, 16)
SBUF_BASE = 16512
SBUF_LIMIT = 229376 - 64


def _dsize(dt):
    return 2 if dt == BF16 else 4


class Buf:
    __slots__ = ("name", "lw", "rd", "dsem", "dcount")

    def __init__(self, name):
        self.name = name
        self.lw = {}
        self.rd = {}
        self.dsem = None
        self.dcount = 0


class Eng:
    def __init__(self, name):
        self.name = name
        self.thunks = []
        self.sem = None
        self.count = 0
        self.seen = {}


class Sched:
    def __init__(self, nc):
        self.nc = nc
        self.stack = ExitStack()
        self.pe = Eng("tensor")
        self.dve = Eng("vector")
        self.act = Eng("scalar")
        self.pool = Eng("gpsimd")
        self.sp = Eng("sync")
        self.engs = [self.pe, self.dve, self.act, self.pool, self.sp]
        for e in self.engs:
            e.sem = self.stack.enter_context(nc.semaphore("S_" + e.name))
        self.nbuf = 0
        self.dma_bufs = []
        self.cur = SBUF_BASE
        self.uid = 0
        self.free_dsems = []
        self.banks = []
        self.bank_i = 0

    def sbuf(self, shape, dtype, name):
        n = 1
        for s in shape[1:]:
            n *= s
        nbytes = (n * _dsize(dtype) + 63) // 64 * 64
        off = self.cur
        self.cur += nbytes
        assert self.cur <= SBUF_LIMIT, f"SBUF overflow at {name}: {self.cur}"
        self.uid += 1
        return self.nc.alloc_sbuf_tensor_at(f"{name}_{self.uid}", list(shape), dtype, offset=off)

    def buf(self, name=None):
        self.nbuf += 1
        return Buf(name or f"b{self.nbuf}")

    def tile(self, shape, dtype, name):
        return self.sbuf(shape, dtype, name), self.buf(name)

    def _dsem(self, b):
        if b.dsem is None:
            if self.free_dsems:
                b.dsem = self.free_dsems.pop()
            else:
                self.uid += 1
                b.dsem = self.stack.enter_context(self.nc.semaphore(f"D{self.uid}"))
            self.dma_bufs.append(b)
        return b.dsem

    def next_bank(self):
        b = self.banks[self.bank_i % len(self.banks)]
        self.bank_i += 1
        return b

    def _deps(self, reads, writes):
        deps = {}
        for b in reads:
            for k, sv in b.lw.items():
                if k not in deps or deps[k][1] < sv[1]:
                    deps[k] = sv
        for b in writes:
            for k, sv in b.lw.items():
                if k not in deps or deps[k][1] < sv[1]:
                    deps[k] = sv
            for k, sv in b.rd.items():
                if k not in deps or deps[k][1] < sv[1]:
                    deps[k] = sv
        return deps

    def _waits(self, eng, deps, skip_self):
        waits = []
        for k, (s, v) in deps.items():
            if skip_self and s is eng.sem:
                continue
            if eng.seen.get(k, 0) >= v:
                continue
            eng.seen[k] = v
            waits.append((s, v))
        return waits

    def _mark(self, reads, writes, s, v, partial):
        k = id(s)
        for b in reads:
            b.rd[k] = (s, v)
        for b in writes:
            if partial:
                b.lw[k] = (s, v)
            else:
                b.lw = {k: (s, v)}
                b.rd = {}

    def op(self, eng, fn, reads=(), writes=(), partial=False):
        deps = self._deps(reads, writes)
        waits = self._waits(eng, deps, eng is self.pe)
        eng.count += 1
        val = eng.count
        sem = eng.sem

        def thunk(e, waits=waits, fn=fn, sem=sem):
            for s, v in waits:
                e.wait_ge(s, v)
            fn(e).then_inc(sem, 1)

        eng.thunks.append(thunk)
        self._mark(reads, writes, sem, val, partial)

    def dma(self, q, fns, sb, reads=(), writes=(), partial=False):
        sem = self._dsem(sb)
        deps = self._deps(reads, writes)
        k = id(sem)
        if sb.dcount > 0 and (k not in deps or deps[k][1] < sb.dcount):
            deps[k] = (sem, sb.dcount)
        waits = self._waits(q, deps, False)
        sb.dcount += 16 * len(fns)
        val = sb.dcount

        def thunk(e, waits=waits, fns=fns, sem=sem):
            for s, v in waits:
                e.wait_ge(s, v)
            for f in fns:
                f(e).then_inc(sem, 16)

        q.thunks.append(thunk)
        self._mark(reads, writes, sem, val, partial)

    def barrier(self):
        finals = [(b.dsem, b.dcount) for b in self.dma_bufs if b.dcount > 0]
        efinal = [(e.sem, e.count) for e in self.engs if e.count > 0]
        for eng in self.engs:
            waits = []
            for s, v in finals + efinal:
                if s is eng.sem and eng is self.pe:
                    continue
                k = id(s)
                if eng.seen.get(k, 0) >= v:
                    continue
                eng.seen[k] = v
                waits.append((s, v))

            def thunk(e, waits=waits):
                for s, v in waits:
                    e.wait_ge(s, v)

            eng.thunks.append(thunk)

    def emit(self):
        with self.nc.Block() as block:
            @block.tensor
            def _(e):
                for t in self.pe.thunks:
                    t(e)

            @block.vector
            def _(e):
                for t in self.dve.thunks:
                    t(e)

            @block.scalar
            def _(e):
                for t in self.act.thunks:
                    t(e)

            @block.gpsimd
            def _(e):
                for t in self.pool.thunks:
                    t(e)

            @block.sync
            def _(e):
                for t in self.sp.thunks:
                    t(e)


def PV_LNG(i, s): return i * 2 + s
def PV_LNB(i, s): return 8 + i * 2 + s
def PV_CW(j, w): return 16 + j * 4 + w
def PV_CB(j): return 24 + j
def PV_GAB(j): return 26 + j
def PV_GXB(j): return 28 + j
def PV_LAM(j): return 30 + j
NPV = 32


def build_program(nlayers=DEPTH, stop=None, dbg=False):
    nc = bass.Bass("TRN2", target_bir_lowering=False)

    declared = []

    def din(name, shape, dt=F32):
        if nlayers < 2 and (name.startswith("rg_") or name.startswith("moe_")):
            return None
        if name.startswith("moe_") and stop is not None and stop < (1, 1):
            return None
        declared.append(name)
        return nc.dram_tensor(name, list(shape), dt, kind="ExternalInput").ap()

    xT_in = din("xT", [D, SEQ])
    cvec = din("cvec", [128, KC])
    pvec_in = din("pvec", [128, NPV * KC])
    relb_in = din("relb", [128, 256])
    ident_in = din("ident", [128, 128])
    sel_in = din("sel", [NE, NE * 128])
    ada_w = din("ada_w", [DEPTH, D, 6 * D])
    ada_b = din("ada_b", [DEPTH, 6 * D])
    attn_w_qkv = din("attn_w_qkv", [2, D, 9 * D])
    attn_w_o = din("attn_w_o", [2, D, D])
    rg_w_in = din("rg_w_in", [2, D, 2 * D])
    rg_ga_w = din("rg_gate_a_w", [2, 8, 256, 256])
    rg_gx_w = din("rg_gate_x_w", [2, 8, 256, 256])
    rg_w_out = din("rg_w_out", [2, D, D])
    ffn_w_in = din("ffn_w_in", [2, D, 2 * DFF])
    ffn_w_out = din("ffn_w_out", [2, DFF, D])
    moe_w_router = din("moe_w_router", [2, D, NE])
    moe_w_in = din("moe_w_in", [2, NE, D, 2 * DFF])
    moe_w_out = din("moe_w_out", [2, NE, DFF, D])
    outT = nc.dram_tensor("outT", [D, SEQ], F32, kind="ExternalOutput").ap()
    XA = nc.dram_tensor("XA", [D, SEQ], F32).ap()
    XB = nc.dram_tensor("XB", [D, SEQ], F32).ap()
    QKV = nc.dram_tensor("QKV", [9 * D, SEQ], BF16).ap()
    OT = nc.dram_tensor("OTs", [D, SEQ], BF16).ap()

    S = Sched(nc)
    for i in range(6):
        t = S.stack.enter_context(nc.psum_tensor(f"bank{i}", [128, 512], F32))
        S.banks.append((t, S.buf(f"bank{i}")))
    pst = [S.stack.enter_context(nc.psum_tensor(f"pst{i}", [128, 8, 128], BF16)) for i in range(2)]
    pstb = [S.buf("pst0"), S.buf("pst1")]

    mod, modb = S.tile([128, DEPTH * 6 * KC], F32, "mod")
    pv, pvb = S.tile([128, NPV * KC], F32, "pv")
    cs, csb = S.tile([128, 2 * KC], F32, "cs")
    relb, relbb = S.tile([128, 256], F32, "relb")
    identf, identfb = S.tile([128, 128], F32, "identf")
    ident, identb = S.tile([128, 128], BF16, "ident")
    ones_b, ones_bb = S.tile([128, 128], BF16, "ones_b")
    ones_f, ones_fb = S.tile([128, 128], F32, "ones_f")
    PERSIST = S.cur

    def mcol(li, slot, k):
        c = (li * 6 + slot) * KC + k
        return mod[:, c:c + 1]

    def pcol(v, k):
        c = v * KC + k
        return pv[:, c:c + 1]

    S.dma(S.sp, [lambda e: e.dma_start(out=pv[:, :], in_=pvec_in[:, :])], pvb, writes=[pvb])
    S.dma(S.sp, [lambda e: e.dma_start(out=relb[:, :], in_=relb_in[:, :])], relbb, writes=[relbb])
    S.dma(S.sp, [lambda e: e.dma_start(out=identf[:, :], in_=ident_in[:, :])], identfb, writes=[identfb])
    S.op(S.dve, lambda e: e.tensor_copy(out=ident[:, :], in_=identf[:, :]), reads=[identfb], writes=[identb])
    S.op(S.dve, lambda e: e.memset(ones_b[:, :], 1.0), writes=[ones_bb])
    S.op(S.dve, lambda e: e.memset(ones_f[:, :], 1.0), writes=[ones_fb])

    def dtrack(n):
        return [S.buf() for _ in range(n)]

    XinB = dtrack(NT)
    XAB = dtrack(NT)
    XBB = dtrack(NT)
    outB = dtrack(NT)
    QKVB = dtrack(1)[0]
    OTB = dtrack(1)[0]

    def xview(X):
        return X.rearrange("(k p) t -> p k t", p=128)

    def phase_ada():
        S.cur = PERSIST
        cact, cactb = S.tile([128, KC], F32, "cact")
        wbufs = [S.tile([128, KC, 512], F32, f"adaw{i}") for i in range(2)]
        brow, browb = S.tile([1, 6 * D], F32, "brow")
        mrow, mrowb = S.tile([1, 6 * D], F32, "mrow")
        S.dma(S.sp, [lambda e: e.dma_start(out=cact[:, :], in_=cvec[:, :])], cactb, writes=[cactb])
        S.op(S.act, lambda e: e.activation(out=cact[:, :], in_=cact[:, :], func=AF.Silu), reads=[cactb], writes=[cactb])
        for j in range(2):
            src = pv[:, PV_LAM(j) * KC:(PV_LAM(j) + 1) * KC]
            dst = cs[:, j * KC:(j + 1) * KC]
            S.op(S.act, lambda e, src=src, dst=dst: e.activation(out=dst, in_=src, func=AF.Exp, scale=-1.0),
                 reads=[pvb], writes=[csb], partial=True)
            S.op(S.act, lambda e, dst=dst: e.activation(out=dst, in_=dst, func=AF.Ln, bias=1.0),
                 reads=[csb], writes=[csb], partial=True)
            S.op(S.dve, lambda e, dst=dst: e.tensor_scalar(out=dst, in0=dst, scalar1=-8.0, scalar2=None, op0=ALU.mult),
                 reads=[csb], writes=[csb], partial=True)
        n = 0
        for li in range(nlayers):
            S.dma(S.sp, [lambda e, li=li: e.dma_start(out=brow[0:1, :], in_=ada_b[li:li + 1, :])], browb, writes=[browb])
            wv = ada_w[li].rearrange("(k p) c -> p k c", p=128)
            for cb in range(24):
                wt, wtb = wbufs[n % 2]
                n += 1
                S.dma(S.sp, [lambda e, wt=wt, wv=wv, cb=cb: e.dma_start(out=wt[:, :, :], in_=wv[:, :, cb * 512:(cb + 1) * 512])],
                      wtb, writes=[wtb])
                ps, psb = S.next_bank()

                def mm(e, ps=ps, wt=wt):
                    for k in range(KC):
                        ins = e.matmul(ps[0:1, :], lhsT=cact[:, k:k + 1], rhs=wt[:, k, :], start=(k == 0), stop=(k == KC - 1))
                    return ins
                S.op(S.pe, mm, reads=[cactb, wtb], writes=[psb])
                S.op(S.dve, lambda e, ps=ps, cb=cb: e.tensor_tensor(out=mrow[0:1, cb * 512:(cb + 1) * 512], in0=ps[0:1, :],
                                                                     in1=brow[0:1, cb * 512:(cb + 1) * 512], op=ALU.add),
                     reads=[psb, browb], writes=[mrowb], partial=True)
            ps, psb = S.next_bank()

            def tr(e, ps=ps):
                for c in range(6 * KC):
                    ins = e.matmul(ps[:, c:c + 1], lhsT=mrow[0:1, c * 128:(c + 1) * 128], rhs=ones_f[0:1, 0:1], start=True, stop=True)
                return ins
            S.op(S.pe, tr, reads=[mrowb, ones_fb], writes=[psb])
            S.op(S.dve, lambda e, ps=ps, li=li: e.tensor_copy(out=mod[:, li * 96:(li + 1) * 96], in_=ps[:, 0:96]),
                 reads=[psb], writes=[modb], partial=True)
            for slot in (1, 2, 4, 5):
                c0 = (li * 6 + slot) * KC
                S.op(S.dve, lambda e, c0=c0: e.tensor_scalar(out=mod[:, c0:c0 + KC], in0=mod[:, c0:c0 + KC], scalar1=1.0, scalar2=None, op0=ALU.add),
                     reads=[modb], writes=[modb], partial=True)
        S.barrier()

    class Ctx:
        pass

    def alloc_common(n_x=2, n_w=3):
        c = Ctx()
        c.xt = [S.tile([128, KC, TT], F32, f"xt{i}") for i in range(n_x)]
        c.xi = 0
        c.u, c.ub = S.tile([128, KC, TT], BF16, "u")
        c.ws = [S.tile([128, KC * 512], BF16, f"ws{i}") for i in range(n_w)]
        c.wi = 0
        return c

    def load_x(c, X, XB_, tt):
        xt, xtb = c.xt[c.xi % len(c.xt)]
        c.xi += 1
        S.dma(S.sp, [lambda e, xt=xt: e.dma_start(out=xt[:, :, :], in_=xview(X)[:, :, tt * TT:(tt + 1) * TT])],
              xtb, reads=[XB_[tt]], writes=[xtb])
        return xt, xtb

    def modulate(c, xt, xtb, li, sub, u=None, ub=None):
        u = c.u if u is None else u
        ub = c.ub if ub is None else ub
        for k in range(KC):
            sc = mcol(li, 3 * sub + 1, k)
            sh = mcol(li, 3 * sub + 0, k)
            if k % 2 == 0:
                S.op(S.dve, lambda e, k=k, sc=sc, sh=sh: e.tensor_scalar(out=u[:, k, :], in0=xt[:, k, :], scalar1=sc, scalar2=sh,
                                                                         op0=ALU.mult, op1=ALU.add),
                     reads=[xtb, modb], writes=[ub], partial=True)
            else:
                S.op(S.act, lambda e, k=k, sc=sc, sh=sh: e.activation(out=u[:, k, :], in_=xt[:, k, :], func=AF.Identity, bias=sh, scale=sc),
                     reads=[xtb, modb], writes=[ub], partial=True)

    def wslab(c, src, nk, ncols):
        t, tb = c.ws[c.wi % len(c.ws)]
        c.wi += 1
        view = t[:, 0:nk * ncols].rearrange("p (k c) -> p k c", k=nk)
        S.dma(S.pool, [lambda e, view=view, src=src: e.dma_start(out=view, in_=src)], tb, writes=[tb])
        return view, tb

    def alloc_ln(c):
        c.sq = [S.tile([128, TT], F32, f"sq{i}") for i in range(2)]
        c.mean, c.meanb = S.tile([128, TT], F32, "mean")
        c.msq, c.msqb = S.tile([128, TT], F32, "msq")
        c.rstd, c.rstdb = S.tile([128, TT], F32, "rstd")
        c.ytmp = [S.tile([128, TT], F32, f"ytmp{i}") for i in range(2)]
        c.ltmp = [S.tile([128, TT], F32, f"ltmp{i}") for i in range(3)]
        c.sqi = 0
        c.yi = 0
        c.li_ = 0

    def out_proj_ln(c, act, actb, kgroups, W, xt, xtb, li, sub, Xout, XoutB, tt):
        nk_total = sum(kgroups)
        Wv = W.rearrange("(k p) c -> p k c", p=128)
        gslot = 3 * sub + 2
        for cg in range(4):
            accs = [S.next_bank() for _ in range(4)]
            k0 = 0
            for gi, nk in enumerate(kgroups):
                wv, wb = wslab(c, Wv[:, k0:k0 + nk, cg * 512:(cg + 1) * 512], nk, 512)
                for m4 in range(4):
                    ps, psb = accs[m4]
                    rb = [wb] + (actb[k0:k0 + nk] if isinstance(actb, list) else [actb])

                    def mm(e, ps=ps, wv=wv, m4=m4, k0=k0, nk=nk):
                        for kk in range(nk):
                            ins = e.matmul(ps[:, :], lhsT=wv[:, kk, m4 * 128:(m4 + 1) * 128], rhs=act[:, k0 + kk, :],
                                           start=(k0 + kk == 0), stop=(k0 + kk == nk_total - 1))
                        return ins
                    S.op(S.pe, mm, reads=rb, writes=[psb], partial=(k0 > 0))
                k0 += nk
            for m4 in range(4):
                m = cg * 4 + m4
                ps, psb = accs[m4]
                yt, ytb = c.ytmp[c.yi % 2]
                c.yi += 1
                gcol = mcol(li, gslot, m)
                S.op(S.act, lambda e, ps=ps, yt=yt, gcol=gcol: e.activation(out=yt[:, :], in_=ps[:, :], func=AF.Identity, scale=gcol),
                     reads=[psb, modb], writes=[ytb])
                S.op(S.dve, lambda e, yt=yt, m=m: e.scalar_tensor_tensor(out=xt[:, m, :], in0=xt[:, m, :], scalar=ALPHA, in1=yt[:, :],
                                                                          op0=ALU.mult, op1=ALU.add),
                     reads=[ytb, xtb], writes=[xtb], partial=True)
        layer_norm(c, xt, xtb, li, sub)
        S.dma(S.sp, [lambda e: e.dma_start(out=xview(Xout)[:, :, tt * TT:(tt + 1) * TT], in_=xt[:, :, :])],
              xtb, reads=[xtb], writes=[XoutB[tt]])

    def layer_norm(c, xt, xtb, li, sub):
        s1, s1b = S.next_bank()
        s2, s2b = S.next_bank()

        def mm1(e):
            for k in range(KC):
                ins = e.matmul(s1[:, :], lhsT=ones_f[:, :], rhs=xt[:, k, :], start=(k == 0), stop=(k == KC - 1))
            return ins
        S.op(S.pe, mm1, reads=[xtb, ones_fb], writes=[s1b])
        for k in range(KC):
            sq, sqb = c.sq[c.sqi % 2]
            c.sqi += 1
            S.op(S.act, lambda e, sq=sq, k=k: e.activation(out=sq[:, :], in_=xt[:, k, :], func=AF.Square), reads=[xtb], writes=[sqb])
            S.op(S.pe, lambda e, sq=sq, k=k: e.matmul(s2[:, :], lhsT=ones_f[:, :], rhs=sq[:, :], start=(k == 0), stop=(k == KC - 1)),
                 reads=[sqb, ones_fb], writes=[s2b], partial=(k > 0))
        S.op(S.act, lambda e: e.activation(out=c.mean[:, :], in_=s1[:, :], func=AF.Copy, scale=1.0 / D), reads=[s1b], writes=[c.meanb])
        S.op(S.dve, lambda e: e.tensor_tensor(out=c.msq[:, :], in0=c.mean[:, :], in1=c.mean[:, :], op=ALU.mult), reads=[c.meanb], writes=[c.msqb])
        S.op(S.dve, lambda e: e.scalar_tensor_tensor(out=c.rstd[:, :], in0=s2[:, :], scalar=1.0 / D, in1=c.msq[:, :], op0=ALU.mult, op1=ALU.subtract),
             reads=[s2b, c.msqb], writes=[c.rstdb])
        S.op(S.dve, lambda e: e.tensor_scalar(out=c.rstd[:, :], in0=c.rstd[:, :], scalar1=LN_EPS, scalar2=None, op0=ALU.add),
             reads=[c.rstdb], writes=[c.rstdb])
        S.op(S.act, lambda e: e.activation(out=c.rstd[:, :], in_=c.rstd[:, :], func=AF.Sqrt), reads=[c.rstdb], writes=[c.rstdb])
        S.op(S.dve, lambda e: e.reciprocal(out=c.rstd[:, :], in_=c.rstd[:, :]), reads=[c.rstdb], writes=[c.rstdb])
        for k in range(KC):
            lt, ltb = c.ltmp[c.li_ % 3]
            c.li_ += 1
            S.op(S.pool, lambda e, lt=lt, k=k: e.tensor_tensor(out=lt[:, :], in0=xt[:, k, :], in1=c.mean[:, :], op=ALU.subtract),
                 reads=[xtb, c.meanb], writes=[ltb])
            S.op(S.dve, lambda e, lt=lt: e.tensor_tensor(out=lt[:, :], in0=lt[:, :], in1=c.rstd[:, :], op=ALU.mult),
                 reads=[ltb, c.rstdb], writes=[ltb])
            g = pcol(PV_LNG(li, sub), k)
            b = pcol(PV_LNB(li, sub), k)
            S.op(S.act, lambda e, lt=lt, k=k, g=g, b=b: e.activation(out=xt[:, k, :], in_=lt[:, :], func=AF.Identity, bias=b, scale=g),
                 reads=[ltb, pvb], writes=[xtb], partial=True)

    def phase_qkv(li, j, Xin, XinB_):
        S.cur = PERSIST
        c = alloc_common()
        stg = [S.tile([128, 4, TT], BF16, f"stg{i}") for i in range(2)]
        Wv = attn_w_qkv[j].rearrange("(k p) c -> p k c", p=128)
        Qv = QKV.rearrange("(c p) t -> p c t", p=128)
        nxt = load_x(c, Xin, XinB_, 0)
        si = 0
        for tt in range(NT):
            xt, xtb = nxt
            modulate(c, xt, xtb, li, 0)
            for sl in range(36):
                if sl == 2 and tt + 1 < NT:
                    nxt = load_x(c, Xin, XinB_, tt + 1)
                wv, wb = wslab(c, Wv[:, :, sl * 512:(sl + 1) * 512], KC, 512)
                st, stb = stg[si % 2]
                si += 1
                for c4 in range(4):
                    ps, psb = S.next_bank()

                    def mm(e, ps=ps, wv=wv, c4=c4):
                        for k in range(KC):
                            ins = e.matmul(ps[:, :], lhsT=wv[:, k, c4 * 128:(c4 + 1) * 128], rhs=c.u[:, k, :], start=(k == 0), stop=(k == KC - 1))
                        return ins
                    S.op(S.pe, mm, reads=[wb, c.ub], writes=[psb])
                    if c4 % 2 == 0:
                        S.op(S.act, lambda e, ps=ps, st=st, c4=c4: e.activation(out=st[:, c4, :], in_=ps[:, :], func=AF.Copy),
                             reads=[psb], writes=[stb], partial=True)
                    else:
                        S.op(S.dve, lambda e, ps=ps, st=st, c4=c4: e.tensor_copy(out=st[:, c4, :], in_=ps[:, :]),
                             reads=[psb], writes=[stb], partial=True)
                S.dma(S.sp, [lambda e, st=st, sl=sl, tt=tt: e.dma_start(out=Qv[:, sl * 4:(sl + 1) * 4, tt * TT:(tt + 1) * TT], in_=st[:, :, :])],
                      stb, reads=[stb], writes=[QKVB], partial=True)
        S.barrier()

    def phase_attn_core():
        S.cur = PERSIST
        qkvt = [S.tile([128, 3, SEQ], BF16, f"qkvt{i}") for i in range(2)]
        vtok = [S.tile([128, 32, 128], BF16, f"vtok{i}") for i in range(2)]
        OL = [S.tile([128, 2, SEQ], F32, f"OL{i}") for i in range(2)]
        oT = [S.tile([128, SEQ], BF16, f"oT{i}") for i in range(2)]
        bhg = [S.tile([128, 2, 128], F32, f"bhg{i}") for i in range(2)]
        ssb = [S.tile([128, 2, 128], F32, f"ssb{i}") for i in range(3)]
        pT = [S.tile([128, 2, 128], BF16, f"pT{i}") for i in range(3)]
        relb3 = relb[:, :].rearrange("p (c q) -> p c q", c=2)
        n_hg = 0
        n_blk = 0
        n_tr = 0
        for h in range(NH):
            ol, olb = OL[h % 2]
            slope = 2.0 ** (-(h + 1) / 2.0)
            for g in range(3):
                d = DILS[g]
                nb = SEQ // d // 128
                qt, qtb = qkvt[n_hg % 2]
                vt, vtb = vtok[n_hg % 2]
                bt, btb = bhg[n_hg % 2]
                n_hg += 1
                rows = [((g * 3 + s) * NH + h) * 128 for s in range(3)]
                S.dma(S.sp, [lambda e, qt=qt, s=s, r=rows[s]: e.dma_start(out=qt[:, s, :], in_=QKV[r:r + 128, :]) for s in range(3)],
                      qtb, reads=[QKVB], writes=[qtb])
                coef = -slope * d / SCALE
                S.op(S.pool, lambda e, bt=bt, coef=coef: e.tensor_scalar(out=bt[:, :, :], in0=relb3, scalar1=coef, scalar2=None, op0=ALU.mult),
                     reads=[relbb], writes=[btb])

                def tok(r, n, d=d):
                    st = n * 128 * d + r
                    return slice(st, st + 127 * d + 1, d)
                for b4 in range(8):
                    half = n_tr % 2
                    n_tr += 1

                    def trf(e, qt=qt, b4=b4, half=half, nb=nb, tok=tok):
                        for i in range(4):
                            bidx = b4 * 4 + i
                            r, n = bidx // nb, bidx % nb
                            ins = e.transpose(out=pst[half][:, i, :], in_=qt[:, 2, tok(r, n)], identity=ident[:, :])
                        return ins
                    S.op(S.pe, trf, reads=[qtb, identb], writes=[pstb[half]])
                    S.op(S.act, lambda e, vt=vt, b4=b4, half=half: e.activation(out=vt[:, b4 * 4:(b4 + 1) * 4, :], in_=pst[half][:, 0:4, :], func=AF.Copy),
                         reads=[pstb[half]], writes=[vtb], partial=True)
                for bidx in range(32):
                    r, n = bidx // nb, bidx % nb
                    c0 = 1 if n == 0 else 0
                    ps, psb = S.next_bank()
                    ps3 = ps[:, 0:256].rearrange("p (c q) -> p c q", c=2)

                    def smm(e, ps3=ps3, qt=qt, r=r, n=n, c0=c0, tok=tok):
                        for cc in range(c0, 2):
                            ins = e.matmul(ps3[:, cc, :], lhsT=qt[:, 1, tok(r, n - 1 + cc)], rhs=qt[:, 0, tok(r, n)], start=True, stop=True)
                        return ins
                    S.op(S.pe, smm, reads=[qtb], writes=[psb])
                    sb_, sbb = ssb[n_blk % 3]
                    p_, pb = pT[n_blk % 3]
                    n_blk += 1
                    S.op(S.dve, lambda e, sb_=sb_, ps3=ps3, bt=bt, c0=c0: e.tensor_tensor(out=sb_[:, c0:2, :], in0=ps3[:, c0:2, :], in1=bt[:, c0:2, :], op=ALU.add),
                         reads=[psb, btb], writes=[sbb])
                    S.op(S.act, lambda e, sb_=sb_, p_=p_, c0=c0: e.activation(out=p_[:, c0:2, :], in_=sb_[:, c0:2, :], func=AF.Exp, scale=SCALE),
                         reads=[sbb], writes=[pb])
                    po, pob = S.next_bank()
                    po3 = po[:, 0:256].rearrange("p (c q) -> p c q", c=2)

                    def pvm(e, po3=po3, vt=vt, p_=p_, c0=c0, bidx=bidx):
                        for cc in range(c0, 2):
                            e.matmul(po3[:, 0, :], lhsT=vt[:, bidx - 1 + cc, :], rhs=p_[:, cc, :], start=(cc == c0), stop=(cc == 1))
                        for cc in range(c0, 2):
                            ins = e.matmul(po3[:, 1, :], lhsT=ones_b[:, :], rhs=p_[:, cc, :], start=(cc == c0), stop=(cc == 1))
                        return ins
                    S.op(S.pe, pvm, reads=[vtb, pb, ones_bb], writes=[pob])
                    dst = ol[:, :, tok(r, n)]
                    if g == 0:
                        S.op(S.act, lambda e, dst=dst, po3=po3: e.activation(out=dst, in_=po3, func=AF.Copy), reads=[pob], writes=[olb], partial=True)
                    else:
                        S.op(S.dve, lambda e, dst=dst, po3=po3: e.tensor_tensor(out=dst, in0=dst, in1=po3, op=ALU.add),
                             reads=[pob, olb], writes=[olb], partial=True)
            o_, ob = oT[h % 2]
            S.op(S.dve, lambda e, ol=ol: e.reciprocal(out=ol[:, 1, :], in_=ol[:, 1, :]), reads=[olb], writes=[olb], partial=True)
            S.op(S.dve, lambda e, ol=ol, o_=o_: e.tensor_tensor(out=o_[:, :], in0=ol[:, 0, :], in1=ol[:, 1, :], op=ALU.mult), reads=[olb], writes=[ob])
            S.dma(S.sp, [lambda e, o_=o_, h=h: e.dma_start(out=OT[h * 128:(h + 1) * 128, :], in_=o_[:, :])], ob, reads=[ob], writes=[OTB], partial=True)
        S.barrier()

    def phase_attn_out(li, j, Xin, XinB_, Xout, XoutB):
        S.cur = PERSIST
        c = alloc_common(n_x=2)
        alloc_ln(c)
        ot = [S.tile([128, KC, TT], BF16, f"ot{i}") for i in range(2)]
        OTv = OT.rearrange("(k p) t -> p k t", p=128)
        for tt in range(NT):
            xt, xtb = load_x(c, Xin, XinB_, tt)
            o_, ob = ot[tt % 2]
            S.dma(S.sp, [lambda e, o_=o_, tt=tt: e.dma_start(out=o_[:, :, :], in_=OTv[:, :, tt * TT:(tt + 1) * TT])], ob, reads=[OTB], writes=[ob])
            out_proj_ln(c, o_, ob, [KC], attn_w_o[j], xt, xtb, li, 0, Xout, XoutB, tt)
        S.barrier()

    def phase_ffn(li, j, Xin, XinB_, Xout, XoutB):
        S.cur = PERSIST
        c = alloc_common(n_x=2)
        alloc_ln(c)
        h, hb_ = S.tile([128, JC, TT], BF16, "h")
        hb = [S.buf() for _ in range(JC)]
        sg = [S.tile([128, TT], BF16, f"sg{i}") for i in range(2)]
        Wv = ffn_w_in[j].rearrange("(k p) c -> p k c", p=128)
        ns = 0
        for tt in range(NT):
            xt, xtb = load_x(c, Xin, XinB_, tt)
            modulate(c, xt, xtb, li, 1)
            for jg in range(11):
                wg, wgb = wslab(c, Wv[:, :, jg * 512:(jg + 1) * 512], KC, 512)
                wu, wub = wslab(c, Wv[:, :, DFF + jg * 512:DFF + (jg + 1) * 512], KC, 512)
                for j4 in range(4):
                    jj = jg * 4 + j4
                    pg, pgb = S.next_bank()
                    pu, pub = S.next_bank()

                    def mmg(e, pg=pg, wg=wg, j4=j4):
                        for k in range(KC):
                            ins = e.matmul(pg[:, :], lhsT=wg[:, k, j4 * 128:(j4 + 1) * 128], rhs=c.u[:, k, :], start=(k == 0), stop=(k == KC - 1))
                        return ins

                    def mmu(e, pu=pu, wu=wu, j4=j4):
                        for k in range(KC):
                            ins = e.matmul(pu[:, :], lhsT=wu[:, k, j4 * 128:(j4 + 1) * 128], rhs=c.u[:, k, :], start=(k == 0), stop=(k == KC - 1))
                        return ins
                    S.op(S.pe, mmg, reads=[wgb, c.ub], writes=[pgb])
                    S.op(S.pe, mmu, reads=[wub, c.ub], writes=[pub])
                    s_, sb_ = sg[ns % 2]
                    ns += 1
                    S.op(S.act, lambda e, s_=s_, pg=pg: e.activation(out=s_[:, :], in_=pg[:, :], func=AF.Silu), reads=[pgb], writes=[sb_])
                    S.op(S.dve, lambda e, s_=s_, pu=pu, jj=jj: e.tensor_tensor(out=h[:, jj, :], in0=s_[:, :], in1=pu[:, :], op=ALU.mult),
                         reads=[sb_, pub], writes=[hb[jj]])
            out_proj_ln(c, h, hb, [11, 11, 11, 11], ffn_w_out[j], xt, xtb, li, 1, Xout, XoutB, tt)
        S.barrier()

    def phase_rglru(li, j, Xin, XinB_, Xout, XoutB):
        S.cur = PERSIST
        c = alloc_common(n_x=1)
        alloc_ln(c)
        gb, gbb_ = S.tile([128, KC, TT], BF16, "gb")
        gbb = [S.buf() for _ in range(KC)]
        y, yb_ = S.tile([128, KC, TT], BF16, "y")
        yb = [S.buf() for _ in range(KC)]
        rec = [S.tile([128, TT + 3], F32, f"rec{i}") for i in range(4)]
        reh, rehb = S.tile([128, KC, 3], F32, "reh")
        hc, hcb = S.tile([128, KC], F32, "hc")
        xc = [S.tile([128, 2, TT], F32, f"xc{i}") for i in range(2)]
        xcb16 = [S.tile([128, 2, TT], BF16, f"xcb{i}") for i in range(2)]
        GA, GAb = S.tile([128, 8, 2, 256], BF16, "GA")
        GX, GXb = S.tile([128, 8, 2, 256], BF16, "GX")
        tmp = {nm: S.tile([128, TT], F32, nm) for nm in ("r", "i", "a", "a2", "bx", "hh")}
        S.dma(S.pool, [lambda e, n=n: e.dma_start(out=GA[:, n, :, :], in_=rg_ga_w[j, n].rearrange("(ic p) o -> p ic o", p=128)) for n in range(8)], GAb, writes=[GAb])
        S.dma(S.pool, [lambda e, n=n: e.dma_start(out=GX[:, n, :, :], in_=rg_gx_w[j, n].rearrange("(ic p) o -> p ic o", p=128)) for n in range(8)], GXb, writes=[GXb])
        S.op(S.dve, lambda e: e.memset(reh[:, :, :], 0.0), writes=[rehb])
        S.op(S.dve, lambda e: e.memset(hc[:, :], 0.0), writes=[hcb])
        Wv = rg_w_in[j].rearrange("(k p) c -> p k c", p=128)
        nrec = 0
        nxc = 0
        for tt in range(NT):
            xt, xtb = load_x(c, Xin, XinB_, tt)
            modulate(c, xt, xtb, li, 0)
            for sl in range(4):
                wv, wb = wslab(c, Wv[:, :, sl * 512:(sl + 1) * 512], KC, 512)
                for c4 in range(4):
                    m = sl * 4 + c4
                    ps, psb = S.next_bank()

                    def mm(e, ps=ps, wv=wv, c4=c4):
                        for k in range(KC):
                            ins = e.matmul(ps[:, :], lhsT=wv[:, k, c4 * 128:(c4 + 1) * 128], rhs=c.u[:, k, :], start=(k == 0), stop=(k == KC - 1))
                        return ins
                    S.op(S.pe, mm, reads=[wb, c.ub], writes=[psb])
                    S.op(S.act, lambda e, ps=ps, m=m: e.activation(out=gb[:, m, :], in_=ps[:, :], func=AF.Gelu_apprx_tanh), reads=[psb], writes=[gbb[m]])
            for sl in range(4):
                wv, wb = wslab(c, Wv[:, :, D + sl * 512:D + (sl + 1) * 512], KC, 512)
                for pr in range(2):
                    xc_, xcb_ = xc[nxc % 2]
                    x16, x16b = xcb16[nxc % 2]
                    nxc += 1
                    nbk = sl * 2 + pr
                    for ic in range(2):
                        m = nbk * 2 + ic
                        c4 = pr * 2 + ic
                        ps, psb = S.next_bank()

                        def mm(e, ps=ps, wv=wv, c4=c4):
                            for k in range(KC):
                                ins = e.matmul(ps[:, :], lhsT=wv[:, k, c4 * 128:(c4 + 1) * 128], rhs=c.u[:, k, :], start=(k == 0), stop=(k == KC - 1))
                            return ins
                        S.op(S.pe, mm, reads=[wb, c.ub], writes=[psb])
                        rc, rcb = rec[nrec % 4]
                        nrec += 1
                        S.op(S.act, lambda e, rc=rc, ps=ps: e.activation(out=rc[:, 3:TT + 3], in_=ps[:, :], func=AF.Copy), reads=[psb], writes=[rcb], partial=True)
                        S.op(S.pool, lambda e, rc=rc, m=m: e.tensor_copy(out=rc[:, 0:3], in_=reh[:, m, :]), reads=[rehb], writes=[rcb], partial=True)
                        S.op(S.act, lambda e, rc=rc, xc_=xc_, ic=ic, m=m: e.activation(out=xc_[:, ic, :], in_=rc[:, 0:TT], func=AF.Identity,
                                                                                     bias=pcol(PV_CB(j), m), scale=pcol(PV_CW(j, 0), m)),
                             reads=[rcb, pvb], writes=[xcb_], partial=True)
                        for w in range(1, 4):
                            eng = S.dve
                            S.op(eng, lambda e, rc=rc, xc_=xc_, ic=ic, m=m, w=w: e.scalar_tensor_tensor(
                                out=xc_[:, ic, :], in0=rc[:, w:w + TT], scalar=pcol(PV_CW(j, w), m), in1=xc_[:, ic, :], op0=ALU.mult, op1=ALU.add),
                                reads=[rcb, pvb, xcb_], writes=[xcb_], partial=True)
                        S.op(S.pool, lambda e, rc=rc, m=m: e.tensor_copy(out=reh[:, m, :], in_=rc[:, TT:TT + 3]), reads=[rcb], writes=[rehb], partial=True)
                    S.op(S.act, lambda e, xc_=xc_, x16=x16: e.activation(out=x16[:, :, :], in_=xc_[:, :, :], func=AF.Copy), reads=[xcb_], writes=[x16b])
                    for oc in range(2):
                        m = nbk * 2 + oc
                        pa, pab = S.next_bank()
                        px, pxb = S.next_bank()

                        def mma(e, pa=pa, x16=x16, nbk=nbk, oc=oc):
                            for ic in range(2):
                                ins = e.matmul(pa[:, :], lhsT=GA[:, nbk, ic, oc * 128:(oc + 1) * 128], rhs=x16[:, ic, :], start=(ic == 0), stop=(ic == 1))
                            return ins

                        def mmx(e, px=px, x16=x16, nbk=nbk, oc=oc):
                            for ic in range(2):
                                ins = e.matmul(px[:, :], lhsT=GX[:, nbk, ic, oc * 128:(oc + 1) * 128], rhs=x16[:, ic, :], start=(ic == 0), stop=(ic == 1))
                            return ins
                        S.op(S.pe, mma, reads=[GAb, x16b], writes=[pab])
                        S.op(S.pe, mmx, reads=[GXb, x16b], writes=[pxb])
                        r_, rb_ = tmp["r"]
                        i_, ib_ = tmp["i"]
                        a_, ab_ = tmp["a"]
                        a2, a2b = tmp["a2"]
                        bx, bxb = tmp["bx"]
                        hh, hhb = tmp["hh"]
                        S.op(S.act, lambda e, pa=pa, r_=r_, m=m: e.activation(out=r_[:, :], in_=pa[:, :], func=AF.Sigmoid, bias=pcol(PV_GAB(j), m)),
                             reads=[pab, pvb], writes=[rb_])
                        S.op(S.act, lambda e, px=px, i_=i_, m=m: e.activation(out=i_[:, :], in_=px[:, :], func=AF.Sigmoid, bias=pcol(PV_GXB(j), m)),
                             reads=[pxb, pvb], writes=[ib_])
                        cscol = cs[:, j * KC + m:j * KC + m + 1]
                        S.op(S.act, lambda e, a_=a_, r_=r_, cscol=cscol: e.activation(out=a_[:, :], in_=r_[:, :], func=AF.Exp, scale=cscol),
                             reads=[rb_, csb], writes=[ab_])
                        S.op(S.pool, lambda e, a2=a2, a_=a_: e.tensor_tensor(out=a2[:, :], in0=a_[:, :], in1=a_[:, :], op=ALU.mult), reads=[ab_], writes=[a2b])
                        S.op(S.dve, lambda e, a2=a2: e.tensor_scalar(out=a2[:, :], in0=a2[:, :], scalar1=-1.0, scalar2=1.0, op0=ALU.mult, op1=ALU.add),
                             reads=[a2b], writes=[a2b])
                        S.op(S.act, lambda e, a2=a2: e.activation(out=a2[:, :], in_=a2[:, :], func=AF.Sqrt), reads=[a2b], writes=[a2b])
                        S.op(S.pool, lambda e, bx=bx, i_=i_, xc_=xc_, oc=oc: e.tensor_tensor(out=bx[:, :], in0=i_[:, :], in1=xc_[:, oc, :], op=ALU.mult),
                             reads=[ib_, xcb_], writes=[bxb])
                        S.op(S.dve, lambda e, bx=bx, a2=a2: e.tensor_tensor(out=bx[:, :], in0=bx[:, :], in1=a2[:, :], op=ALU.mult), reads=[bxb, a2b], writes=[bxb])
                        S.op(S.dve, lambda e, hh=hh, a_=a_, bx=bx, m=m: e.tensor_tensor_scan(out=hh[:, :], data0=a_[:, :], data1=bx[:, :], initial=hc[:, m:m + 1],
                                                                                            op0=ALU.mult, op1=ALU.add),
                             reads=[ab_, bxb, hcb], writes=[hhb])
                        S.op(S.dve, lambda e, hh=hh, m=m: e.tensor_copy(out=hc[:, m:m + 1], in_=hh[:, TT - 1:TT]), reads=[hhb], writes=[hcb], partial=True)
                        S.op(S.dve, lambda e, hh=hh, m=m: e.tensor_tensor(out=y[:, m, :], in0=gb[:, m, :], in1=hh[:, :], op=ALU.mult),
                             reads=[hhb, gbb[m]], writes=[yb[m]])
            out_proj_ln(c, y, yb, [KC], rg_w_out[j], xt, xtb, li, 0, Xout, XoutB, tt)
        S.barrier()

    def phase_moe(li, j, Xin, XinB_, Xout, XoutB):
        S.cur = PERSIST
        c = alloc_common(n_x=1, n_w=3)
        alloc_ln(c)
        u32 = [S.tile([128, TT], F32, f"u32_{i}") for i in range(2)]
        h, hb_ = S.tile([128, JC, TT], BF16, "h")
        hb = [S.buf() for _ in range(JC)]
        sg = [S.tile([128, TT], BF16, f"sg{i}") for i in range(2)]
        wr, wrb = S.tile([128, KC, NE], F32, "wr")
        lg, lgb = S.tile([128, 4, NE], F32, "lg")
        mx, mxb = S.tile([128, 4, 8], F32, "mx")
        wtk, wtkb = S.tile([128, 4, NE], F32, "wtk")
        den, denb = S.tile([128, 4, 1], F32, "den")
        wT, wTb = S.tile([NE, TT], F32, "wT")
        sel, selb = S.tile([NE, NE, 128], F32, "sel")
        wB, wBb_ = S.tile([128, NE, TT], F32, "wB")
        wBb = [S.buf() for _ in range(NE)]
        S.dma(S.sp, [lambda e: e.dma_start(out=wr[:, :, :], in_=moe_w_router[j].rearrange("(k p) n -> p k n", p=128))], wrb, writes=[wrb])
        S.dma(S.sp, [lambda e: e.dma_start(out=sel[:, :, :], in_=sel_in.rearrange("k (e m) -> k e m", e=NE))], selb, writes=[selb])
        ns = 0
        for tt in range(NT):
            xt, xtb = load_x(c, Xin, XinB_, tt)
            pls = [S.next_bank() for _ in range(4)]
            for k in range(KC):
                sc = mcol(li, 4, k)
                sh = mcol(li, 3, k)
                uk, ukb = u32[k % 2]
                S.op(S.dve, lambda e, k=k, sc=sc, sh=sh, uk=uk: e.tensor_scalar(out=uk[:, :], in0=xt[:, k, :], scalar1=sc, scalar2=sh, op0=ALU.mult, op1=ALU.add),
                     reads=[xtb, modb], writes=[ukb])
                S.op(S.act, lambda e, k=k, uk=uk: e.activation(out=c.u[:, k, :], in_=uk[:, :], func=AF.Copy), reads=[ukb], writes=[c.ub], partial=True)
                for s4 in range(4):
                    pl, plb = pls[s4]
                    S.op(S.pe, lambda e, pl=pl, uk=uk, k=k, s4=s4: e.matmul(pl[:, 0:NE], lhsT=uk[:, s4 * 128:(s4 + 1) * 128], rhs=wr[:, k, :], start=(k == 0), stop=(k == KC - 1)),
                         reads=[ukb, wrb], writes=[plb], partial=(k > 0))
            for k in range(KC):
                S.op(S.pool, lambda e, k=k: e.tensor_scalar(out=xt[:, k, :], in0=xt[:, k, :], scalar1=ALPHA, scalar2=None, op0=ALU.mult),
                     reads=[xtb], writes=[xtb], partial=True)
            for s4 in range(4):
                pl, plb = pls[s4]
                S.op(S.dve, lambda e, pl=pl, s4=s4: e.tensor_copy(out=lg[:, s4, :], in_=pl[:, 0:NE]), reads=[plb], writes=[lgb], partial=True)
            for s4 in range(4):
                S.op(S.dve, lambda e, s4=s4: e.max(out=mx[:, s4, :], in_=lg[:, s4, :]), reads=[lgb], writes=[mxb], partial=True)
            for s4 in range(4):
                S.op(S.dve, lambda e, s4=s4: e.tensor_scalar(out=wtk[:, s4, :], in0=lg[:, s4, :], scalar1=mx[:, s4, 0:1], scalar2=None, op0=ALU.subtract),
                     reads=[lgb, mxb], writes=[wtkb], partial=True)
            S.op(S.act, lambda e: e.activation(out=wtk[:, :, :], in_=wtk[:, :, :], func=AF.Exp), reads=[wtkb], writes=[wtkb])
            for s4 in range(4):
                S.op(S.dve, lambda e, s4=s4: e.scalar_tensor_tensor(out=wtk[:, s4, :], in0=lg[:, s4, :], scalar=mx[:, s4, 1:2], in1=wtk[:, s4, :],
                                                                    op0=ALU.is_ge, op1=ALU.mult),
                     reads=[lgb, mxb, wtkb], writes=[wtkb], partial=True)
            S.op(S.dve, lambda e: e.tensor_tensor(out=den[:, :, :], in0=mx[:, :, 1:2], in1=mx[:, :, 0:1], op=ALU.subtract), reads=[mxb], writes=[denb])
            S.op(S.act, lambda e: e.activation(out=den[:, :, :], in_=den[:, :, :], func=AF.Exp), reads=[denb], writes=[denb])
            S.op(S.dve, lambda e: e.tensor_scalar(out=den[:, :, :], in0=den[:, :, :], scalar1=1.0, scalar2=None, op0=ALU.add), reads=[denb], writes=[denb])
            S.op(S.dve, lambda e: e.reciprocal(out=den[:, :, :], in_=den[:, :, :]), reads=[denb], writes=[denb])
            for s4 in range(4):
                S.op(S.dve, lambda e, s4=s4: e.tensor_scalar(out=wtk[:, s4, :], in0=wtk[:, s4, :], scalar1=den[:, s4, 0:1], scalar2=None, op0=ALU.mult),
                     reads=[denb, wtkb], writes=[wtkb], partial=True)
            pt, ptb = S.next_bank()

            def trw(e, pt=pt):
                for s4 in range(4):
                    ins = e.transpose(out=pt[0:NE, s4 * 128:(s4 + 1) * 128], in_=wtk[:, s4, :], identity=identf[:, :])
                return ins
            S.op(S.pe, trw, reads=[wtkb, identfb], writes=[ptb])
            S.op(S.dve, lambda e, pt=pt: e.tensor_copy(out=wT[:, :], in_=pt[0:NE, :]), reads=[ptb], writes=[wTb])
            for ex in range(NE):
                pb_, pbb = S.next_bank()
                S.op(S.pe, lambda e, pb_=pb_, ex=ex: e.matmul(pb_[:, :], lhsT=sel[:, ex, :], rhs=wT[:, :], start=True, stop=True), reads=[selb, wTb], writes=[pbb])
                S.op(S.act, lambda e, pb_=pb_, ex=ex: e.activation(out=wB[:, ex, :], in_=pb_[:, :], func=AF.Copy), reads=[pbb], writes=[wBb[ex]])
            for ex in range(NE):
                Wv = moe_w_in[j, ex].rearrange("(k p) c -> p k c", p=128)
                for jg in range(11):
                    wg, wgb = wslab(c, Wv[:, :, jg * 512:(jg + 1) * 512], KC, 512)
                    wu, wub = wslab(c, Wv[:, :, DFF + jg * 512:DFF + (jg + 1) * 512], KC, 512)
                    for j4 in range(4):
                        jj = jg * 4 + j4
                        pg, pgb = S.next_bank()
                        pu, pub = S.next_bank()

                        def mmg(e, pg=pg, wg=wg, j4=j4):
                            for k in range(KC):
                                ins = e.matmul(pg[:, :], lhsT=wg[:, k, j4 * 128:(j4 + 1) * 128], rhs=c.u[:, k, :], start=(k == 0), stop=(k == KC - 1))
                            return ins

                        def mmu(e, pu=pu, wu=wu, j4=j4):
                            for k in range(KC):
                                ins = e.matmul(pu[:, :], lhsT=wu[:, k, j4 * 128:(j4 + 1) * 128], rhs=c.u[:, k, :], start=(k == 0), stop=(k == KC - 1))
                            return ins
                        S.op(S.pe, mmg, reads=[wgb, c.ub], writes=[pgb])
                        S.op(S.pe, mmu, reads=[wub, c.ub], writes=[pub])
                        s_, sb_ = sg[ns % 2]
                        ns += 1
                        S.op(S.act, lambda e, s_=s_, pg=pg: e.activation(out=s_[:, :], in_=pg[:, :], func=AF.Silu), reads=[pgb], writes=[sb_])
                        S.op(S.dve, lambda e, s_=s_, pu=pu, jj=jj: e.tensor_tensor(out=h[:, jj, :], in0=s_[:, :], in1=pu[:, :], op=ALU.mult),
                             reads=[sb_, pub], writes=[hb[jj]])
                Wo = moe_w_out[j, ex].rearrange("(k p) c -> p k c", p=128)
                for cg in range(4):
                    accs = [S.next_bank() for _ in range(4)]
                    for gi in range(4):
                        wv, wb = wslab(c, Wo[:, gi * 11:(gi + 1) * 11, cg * 512:(cg + 1) * 512], 11, 512)
                        for m4 in range(4):
                            ps, psb = accs[m4]

                            def mm(e, ps=ps, wv=wv, m4=m4, gi=gi):
                                for kk in range(11):
                                    ins = e.matmul(ps[:, :], lhsT=wv[:, kk, m4 * 128:(m4 + 1) * 128], rhs=h[:, gi * 11 + kk, :],
                                                   start=(gi == 0 and kk == 0), stop=(gi == 3 and kk == 10))
                                return ins
                            S.op(S.pe, mm, reads=[wb] + hb[gi * 11:(gi + 1) * 11], writes=[psb], partial=(gi > 0))
                    for m4 in range(4):
                        m = cg * 4 + m4
                        ps, psb = accs[m4]
                        yt, ytb = c.ytmp[c.yi % 2]
                        c.yi += 1
                        S.op(S.dve, lambda e, ps=ps, yt=yt, ex=ex: e.tensor_tensor(out=yt[:, :], in0=ps[:, :], in1=wB[:, ex, :], op=ALU.mult),
                             reads=[psb, wBb[ex]], writes=[ytb])
                        S.op(S.dve, lambda e, yt=yt, m=m: e.scalar_tensor_tensor(out=xt[:, m, :], in0=yt[:, :], scalar=mcol(li, 5, m), in1=xt[:, m, :],
                                                                                  op0=ALU.mult, op1=ALU.add),
                             reads=[ytb, xtb, modb], writes=[xtb], partial=True)
            layer_norm(c, xt, xtb, li, 1)
            S.dma(S.sp, [lambda e, xt=xt, tt=tt: e.dma_start(out=xview(Xout)[:, :, tt * TT:(tt + 1) * TT], in_=xt[:, :, :])],
                  xtb, reads=[xtb], writes=[XoutB[tt]])
        S.barrier()

    phase_ada()
    if dbg == "ada":
        S.dma(S.sp, [lambda e: e.dma_start(out=outT[0:128, 0:DEPTH * 96], in_=mod[:, :])], modb, reads=[modb])
        S.dma(S.sp, [lambda e: e.dma_start(out=outT[128:256, 0:2 * KC], in_=cs[:, :])], csb, reads=[csb])
        S.barrier()
        S.emit()
        S.declared = declared
        return nc, S
    steps = []
    for li in range(nlayers):
        steps.append((li, 0))
        steps.append((li, 1))
    if stop is not None:
        steps = [s for s in steps if s <= stop]
    cur, curB = xT_in, XinB
    for si, (li, sub) in enumerate(steps):
        last = si == len(steps) - 1
        if last:
            nxt, nxtB = outT, outB
        elif cur is XA:
            nxt, nxtB = XB, XBB
        else:
            nxt, nxtB = XA, XAB
        j = li // 2
        if sub == 0 and li % 2 == 0:
            phase_qkv(li, j, cur, curB)
            if dbg in ("qkv", "core"):
                if dbg == "core":
                    phase_attn_core()
                S.cur = PERSIST
                tb16, tb16b = S.tile([128, SEQ], BF16, "dbg16")
                tb32, tb32b = S.tile([128, SEQ], F32, "dbg32")
                rows = [0, 2048, 4096] if dbg == "qkv" else [0, 128, 1920]
                for ri, r in enumerate(rows):
                    src = QKV if dbg == "qkv" else OT
                    S.dma(S.sp, [lambda e, r=r, src=src: e.dma_start(out=tb16[:, :], in_=src[r:r + 128, :])], tb16b, reads=[QKVB, OTB], writes=[tb16b])
                    S.op(S.act, lambda e: e.activation(out=tb32[:, :], in_=tb16[:, :], func=AF.Copy), reads=[tb16b], writes=[tb32b])
                    S.dma(S.sp, [lambda e, ri=ri: e.dma_start(out=outT[ri * 128:(ri + 1) * 128, :], in_=tb32[:, :])], tb32b, reads=[tb32b])
                S.barrier()
                S.emit()
                S.declared = declared
                return nc, S
            phase_attn_core()
            phase_attn_out(li, j, cur, curB, nxt, nxtB)
        elif sub == 0:
            phase_rglru(li, j, cur, curB, nxt, nxtB)
        elif li % 2 == 0:
            phase_ffn(li, j, cur, curB, nxt, nxtB)
        else:
            phase_moe(li, j, cur, curB, nxt, nxtB)
        cur, curB = nxt, nxtB
    S.barrier()
    S.emit()
    S.declared = declared
    return nc, S


def host_consts():
    qi = np.arange(128)[None, :]
    kj = np.arange(128)[:, None]
    prev = np.where(kj >= qi, (qi + 128 - kj).astype(np.float32), BIGREL)
    curr = np.where(kj <= qi, (qi - kj).astype(np.float32), BIGREL)
    relb = np.concatenate([prev, curr], axis=1).astype(np.float32)
    ident = np.eye(128, dtype=np.float32)
    sel = np.zeros((NE, NE, 128), np.float32)
    for e_ in range(NE):
        sel[e_, e_, :] = 1.0
    return relb, ident, sel.reshape(NE, NE * 128)


def col_layout(v):
    return np.ascontiguousarray(np.asarray(v, np.float32).reshape(KC, 128).T)


def make_in_maps(inputs, cores, declared=None):
    f = lambda k: np.ascontiguousarray(np.asarray(inputs[k], dtype=np.float32))
    relb, ident, sel = host_consts()
    pv = np.zeros((128, NPV, KC), np.float32)
    for i in range(DEPTH):
        for s in range(2):
            pv[:, PV_LNG(i, s)] = col_layout(inputs["ln_g"][i, s])
            pv[:, PV_LNB(i, s)] = col_layout(inputs["ln_b"][i, s])
    for j in range(2):
        for w in range(4):
            pv[:, PV_CW(j, w)] = col_layout(inputs["rg_conv_w"][j, w])
        pv[:, PV_CB(j)] = col_layout(inputs["rg_conv_b"][j])
        pv[:, PV_GAB(j)] = col_layout(inputs["rg_gate_a_b"][j])
        pv[:, PV_GXB(j)] = col_layout(inputs["rg_gate_x_b"][j])
        pv[:, PV_LAM(j)] = col_layout(inputs["rg_lambda"][j])
    pv = np.ascontiguousarray(pv.reshape(128, NPV * KC))
    shared = {k: f(k) for k in (kk for kk in ("ada_w", "ada_b", "attn_w_qkv", "attn_w_o", "rg_w_in", "rg_gate_a_w", "rg_gate_x_w", "rg_w_out",
                                "ffn_w_in", "ffn_w_out", "moe_w_router", "moe_w_in", "moe_w_out") if declared is None or kk in declared)}
    x = np.asarray(inputs["x"], np.float32)
    cc = np.asarray(inputs["c"], np.float32)
    maps = []
    for b in cores:
        m = dict(shared)
        m["xT"] = np.ascontiguousarray(x[b].T)
        m["cvec"] = col_layout(cc[b])
        m["pvec"] = pv
        m["relb"] = relb
        m["ident"] = ident
        m["sel"] = sel
        if declared is not None:
            m = {k: v for k, v in m.items() if k in declared}
        maps.append(m)
    return maps


_CACHE = {}


def kernel(**inputs):
    if "nc" not in _CACHE:
        _CACHE["nc"] = build_program()
    nc, S = _CACHE["nc"]
    maps = make_in_maps(inputs, list(range(8)))
    res = run_bass_kernel_spmd(nc, maps, core_ids=list(range(8)))
    out = np.stack([np.ascontiguousarray(r["outT"].T) for r in res.results], axis=0)
    return out.astype(np.float32)
```

```python
from contextlib import ExitStack

import numpy as np

import concourse.bass as bass
import concourse.mybir as mybir
from concourse.bass_utils import run_bass_kernel_spmd

F32 = mybir.dt.float32
BF16 = mybir.dt.bfloat16
ALU = mybir.AluOpType
AF = mybir.ActivationFunctionType

D = 2048
SEQ = 4096
KC = 16
TT = 512
NT = SEQ // TT
DFF = 5632
JC = DFF // 128
NH = 16
NE = 8
DEPTH = 4
ALPHA = float((2 * DEPTH) ** 0.25)
LN_EPS = 1e-5
SCALE = float(128 ** -0.5)
BIGREL = 1.0e5
DILS = (1, 4, 16)
SBUF_BASE = 16512
SBUF_LIMIT = 229376 - 64


def _dsize(dt):
    return 2 if dt == BF16 else 4


class Buf:
    __slots__ = ("name", "lw", "rd", "dsem", "dcount")

    def __init__(self, name):
        self.name = name
        self.lw = {}
        self.rd = {}
        self.dsem = None
        self.dcount = 0


class Eng:
    def __init__(self, name):
        self.name = name
        self.thunks = []
        self.sem = None
        self.count = 0
        self.seen = {}


class Sched:
    def __init__(self, nc):
        self.nc = nc
        self.stack = ExitStack()
        self.pe = Eng("tensor")
        self.dve = Eng("vector")
        self.act = Eng("scalar")
        self.pool = Eng("gpsimd")
        self.sp = Eng("sync")
        self.engs = [self.pe, self.dve, self.act, self.pool, self.sp]
        for e in self.engs:
            e.sem = self.stack.enter_context(nc.semaphore("S_" + e.name))
        self.nbuf = 0
        self.dma_bufs = []
        self.cur = SBUF_BASE
        self.uid = 0
        self.free_dsems = []
        self.banks = []
        self.bank_i = 0

    def sbuf(self, shape, dtype, name):
        n = 1
        for s in shape[1:]:
            n *= s
        nbytes = (n * _dsize(dtype) + 63) // 64 * 64
        off = self.cur
        self.cur += nbytes
        assert self.cur <= SBUF_LIMIT, f"SBUF overflow at {name}: {self.cur}"
        self.uid += 1
        return self.nc.alloc_sbuf_tensor_at(f"{name}_{self.uid}", list(shape), dtype, offset=off)

    def buf(self, name=None):
        self.nbuf += 1
        return Buf(name or f"b{self.nbuf}")

    def tile(self, shape, dtype, name):
        return self.sbuf(shape, dtype, name), self.buf(name)

    def _dsem(self, b):
        if b.dsem is None:
            if self.free_dsems:
                b.dsem = self.free_dsems.pop()
            else:
                self.uid += 1
                b.dsem = self.stack.enter_context(self.nc.semaphore(f"D{self.uid}"))
            self.dma_bufs.append(b)
        return b.dsem

    def next_bank(self):
        b = self.banks[self.bank_i % len(self.banks)]
        self.bank_i += 1
        return b

    def _deps(self, reads, writes):
        deps = {}
        for b in reads:
            for k, sv in b.lw.items():
                if k not in deps or deps[k][1] < sv[1]:
                    deps[k] = sv
        for b in writes:
            for k, sv in b.lw.items():
                if k not in deps or deps[k][1] < sv[1]:
                    deps[k] = sv
            for k, sv in b.rd.items():
                if k not in deps or deps[k][1] < sv[1]:
                    deps[k] = sv
        return deps

    def _waits(self, eng, deps, skip_self):
        waits = []
        for k, (s, v) in deps.items():
            if skip_self and s is eng.sem:
                continue
            if eng.seen.get(k, 0) >= v:
                continue
            eng.seen[k] = v
            waits.append((s, v))
        return waits

    def _mark(self, reads, writes, s, v, partial):
        k = id(s)
        for b in reads:
            b.rd[k] = (s, v)
        for b in writes:
            if partial:
                b.lw[k] = (s, v)
            else:
                b.lw = {k: (s, v)}
                b.rd = {}

    def op(self, eng, fn, reads=(), writes=(), partial=False):
        deps = self._deps(reads, writes)
        waits = self._waits(eng, deps, eng is self.pe)
        eng.count += 1
        val = eng.count
        sem = eng.sem

        def thunk(e, waits=waits, fn=fn, sem=sem):
            for s, v in waits:
                e.wait_ge(s, v)
            fn(e).then_inc(sem, 1)

        eng.thunks.append(thunk)
        self._mark(reads, writes, sem, val, partial)

    def dma(self, q, fns, sb, reads=(), writes=(), partial=False):
        sem = self._dsem(sb)
        deps = self._deps(reads, writes)
        k = id(sem)
        if sb.dcount > 0 and (k not in deps or deps[k][1] < sb.dcount):
            deps[k] = (sem, sb.dcount)
        waits = self._waits(q, deps, False)
        sb.dcount += 16 * len(fns)
        val = sb.dcount

        def thunk(e, waits=waits, fns=fns, sem=sem):
            for s, v in waits:
                e.wait_ge(s, v)
            for f in fns:
                f(e).then_inc(sem, 16)

        q.thunks.append(thunk)
        self._mark(reads, writes, sem, val, partial)

    def barrier(self):
        finals = [(b.dsem, b.dcount) for b in self.dma_bufs if b.dcount > 0]
        efinal = [(e.sem, e.count) for e in self.engs if e.count > 0]
        for eng in self.engs:
            waits = []
            for s, v in finals + efinal:
                if s is eng.sem and eng is self.pe:
                    continue
                k = id(s)
                if eng.seen.get(k, 0) >= v:
                    continue
                eng.seen[k] = v
                waits.append((s, v))

            def thunk(e, waits=waits):
                for s, v in waits:
                    e.wait_ge(s, v)

            eng.thunks.append(thunk)

    def emit(self):
        with self.nc.Block() as block:
            @block.tensor
            def _(e):
                for t in self.pe.thunks:
                    t(e)

            @block.vector
            def _(e):
                for t in self.dve.thunks:
                    t(e)

            @block.scalar
            def _(e):
                for t in self.act.thunks:
                    t(e)

            @block.gpsimd
            def _(e):
                for t in self.pool.thunks:
                    t(e)

            @block.sync
            def _(e):
                for t in self.sp.thunks:
                    t(e)


def PV_LNG(i, s): return i * 2 + s
def PV_LNB(i, s): return 8 + i * 2 + s
def PV_CW(j, w): return 16 + j * 4 + w
def PV_CB(j): return 24 + j
def PV_GAB(j): return 26 + j
def PV_GXB(j): return 28 + j
def PV_LAM(j): return 30 + j
NPV = 32


def build_program(nlayers=DEPTH, stop=None, dbg=False):
    nc = bass.Bass("TRN2", target_bir_lowering=False)

    declared = []

    def din(name, shape, dt=F32):
        if nlayers < 2 and (name.startswith("rg_") or name.startswith("moe_")):
            return None
        if name.startswith("moe_") and stop is not None and stop < (1, 1):
            return None
        declared.append(name)
        return nc.dram_tensor(name, list(shape), dt, kind="ExternalInput").ap()

    xT_in = din("xT", [D, SEQ])
    cvec = din("cvec", [128, KC])
    pvec_in = din("pvec", [128, NPV * KC])
    relb_in = din("relb", [128, 256])
    ident_in = din("ident", [128, 128])
    sel_in = din("sel", [NE, NE * 128])
    ada_w = din("ada_w", [DEPTH, D, 6 * D])
    ada_b = din("ada_b", [DEPTH, 6 * D])
    attn_w_qkv = din("attn_w_qkv", [2, D, 9 * D])
    attn_w_o = din("attn_w_o", [2, D, D])
    rg_w_in = din("rg_w_in", [2, D, 2 * D])
    rg_ga_w = din("rg_gate_a_w", [2, 8, 256, 256])
    rg_gx_w = din("rg_gate_x_w", [2, 8, 256, 256])
    rg_w_out = din("rg_w_out", [2, D, D])
    ffn_w_in = din("ffn_w_in", [2, D, 2 * DFF])
    ffn_w_out = din("ffn_w_out", [2, DFF, D])
    moe_w_router = din("moe_w_router", [2, D, NE])
    moe_w_in = din("moe_w_in", [2, NE, D, 2 * DFF])
    moe_w_out = din("moe_w_out", [2, NE, DFF, D])
    outT = nc.dram_tensor("outT", [D, SEQ], F32, kind="ExternalOutput").ap()
    XA = nc.dram_tensor("XA", [D, SEQ], F32).ap()
    XB = nc.dram_tensor("XB", [D, SEQ], F32).ap()
    QKV = nc.dram_tensor("QKV", [9 * D, SEQ], BF16).ap()
    OT = nc.dram_tensor("OTs", [D, SEQ], BF16).ap()

    S = Sched(nc)
    for i in range(6):
        t = S.stack.enter_context(nc.psum_tensor(f"bank{i}", [128, 512], F32))
        S.banks.append((t, S.buf(f"bank{i}")))
    pst = [S.stack.enter_context(nc.psum_tensor(f"pst{i}", [128, 8, 128], BF16)) for i in range(2)]
    pstb = [S.buf("pst0"), S.buf("pst1")]

    mod, modb = S.tile([128, DEPTH * 6 * KC], F32, "mod")
    pv, pvb = S.tile([128, NPV * KC], F32, "pv")
    cs, csb = S.tile([128, 2 * KC], F32, "cs")
    relb, relbb = S.tile([128, 256], F32, "relb")
    identf, identfb = S.tile([128, 128], F32, "identf")
    ident, identb = S.tile([128, 128], BF16, "ident")
    ones_b, ones_bb = S.tile([128, 128], BF16, "ones_b")
    ones_f, ones_fb = S.tile([128, 128], F32, "ones_f")
    PERSIST = S.cur

    def mcol(li, slot, k):
        c = (li * 6 + slot) * KC + k
        return mod[:, c:c + 1]

    def pcol(v, k):
        c = v * KC + k
        return pv[:, c:c + 1]

    S.dma(S.sp, [lambda e: e.dma_start(out=pv[:, :], in_=pvec_in[:, :])], pvb, writes=[pvb])
    S.dma(S.sp, [lambda e: e.dma_start(out=relb[:, :], in_=relb_in[:, :])], relbb, writes=[relbb])
    S.dma(S.sp, [lambda e: e.dma_start(out=identf[:, :], in_=ident_in[:, :])], identfb, writes=[identfb])
    S.op(S.dve, lambda e: e.tensor_copy(out=ident[:, :], in_=identf[:, :]), reads=[identfb], writes=[identb])
    S.op(S.dve, lambda e: e.memset(ones_b[:, :], 1.0), writes=[ones_bb])
    S.op(S.dve, lambda e: e.memset(ones_f[:, :], 1.0), writes=[ones_fb])

    def dtrack(n):
        return [S.buf() for _ in range(n)]

    XinB = dtrack(NT)
    XAB = dtrack(NT)
    XBB = dtrack(NT)
    outB = dtrack(NT)
    QKVB = dtrack(1)[0]
    OTB = dtrack(1)[0]

    def xview(X):
        return X.rearrange("(k p) t -> p k t", p=128)

    def phase_ada():
        S.cur = PERSIST
        cact, cactb = S.tile([128, KC], F32, "cact")
        wbufs = [S.tile([128, KC, 512], F32, f"adaw{i}") for i in range(2)]
        brow, browb = S.tile([1, 6 * D], F32, "brow")
        mrow, mrowb = S.tile([1, 6 * D], F32, "mrow")
        S.dma(S.sp, [lambda e: e.dma_start(out=cact[:, :], in_=cvec[:, :])], cactb, writes=[cactb])
        S.op(S.act, lambda e: e.activation(out=cact[:, :], in_=cact[:, :], func=AF.Silu), reads=[cactb], writes=[cactb])
        for j in range(2):
            src = pv[:, PV_LAM(j) * KC:(PV_LAM(j) + 1) * KC]
            dst = cs[:, j * KC:(j + 1) * KC]
            S.op(S.act, lambda e, src=src, dst=dst: e.activation(out=dst, in_=src, func=AF.Exp, scale=-1.0),
                 reads=[pvb], writes=[csb], partial=True)
            S.op(S.act, lambda e, dst=dst: e.activation(out=dst, in_=dst, func=AF.Ln, bias=1.0),
                 reads=[csb], writes=[csb], partial=True)
            S.op(S.dve, lambda e, dst=dst: e.tensor_scalar(out=dst, in0=dst, scalar1=-8.0, scalar2=None, op0=ALU.mult),
                 reads=[csb], writes=[csb], partial=True)
        n = 0
        for li in range(nlayers):
            S.dma(S.sp, [lambda e, li=li: e.dma_start(out=brow[0:1, :], in_=ada_b[li:li + 1, :])], browb, writes=[browb])
            wv = ada_w[li].rearrange("(k p) c -> p k c", p=128)
            for cb in range(24):
                wt, wtb = wbufs[n % 2]
                n += 1
                S.dma(S.sp, [lambda e, wt=wt, wv=wv, cb=cb: e.dma_start(out=wt[:, :, :], in_=wv[:, :, cb * 512:(cb + 1) * 512])],
                      wtb, writes=[wtb])
                ps, psb = S.next_bank()

                def mm(e, ps=ps, wt=wt):
                    for k in range(KC):
                        ins = e.matmul(ps[0:1, :], lhsT=cact[:, k:k + 1], rhs=wt[:, k, :], start=(k == 0), stop=(k == KC - 1))
                    return ins
                S.op(S.pe, mm, reads=[cactb, wtb], writes=[psb])
                S.op(S.dve, lambda e, ps=ps, cb=cb: e.tensor_tensor(out=mrow[0:1, cb * 512:(cb + 1) * 512], in0=ps[0:1, :],
                                                                     in1=brow[0:1, cb * 512:(cb + 1) * 512], op=ALU.add),
                     reads=[psb, browb], writes=[mrowb], partial=True)
            ps, psb = S.next_bank()

            def tr(e, ps=ps):
                for c in range(6 * KC):
                    ins = e.matmul(ps[:, c:c + 1], lhsT=mrow[0:1, c * 128:(c + 1) * 128], rhs=ones_f[0:1, 0:1], start=True, stop=True)
                return ins
            S.op(S.pe, tr, reads=[mrowb, ones_fb], writes=[psb])
            S.op(S.dve, lambda e, ps=ps, li=li: e.tensor_copy(out=mod[:, li * 96:(li + 1) * 96], in_=ps[:, 0:96]),
                 reads=[psb], writes=[modb], partial=True)
            for slot in (1, 2, 4, 5):
                c0 = (li * 6 + slot) * KC
                S.op(S.dve, lambda e, c0=c0: e.tensor_scalar(out=mod[:, c0:c0 + KC], in0=mod[:, c0:c0 + KC], scalar1=1.0, scalar2=None, op0=ALU.add),
                     reads=[modb], writes=[modb], partial=True)
        S.barrier()

    class Ctx:
        pass

    def alloc_common(n_x=2, n_w=3):
        c = Ctx()
        c.xt = [S.tile([128, KC, TT], F32, f"xt{i}") for i in range(n_x)]
        c.xi = 0
        c.u, c.ub = S.tile([128, KC, TT], BF16, "u")
        c.ws = [S.tile([128, KC * 512], BF16, f"ws{i}") for i in range(n_w)]
        c.wi = 0
        return c

    def load_x(c, X, XB_, tt):
        xt, xtb = c.xt[c.xi % len(c.xt)]
        c.xi += 1
        S.dma(S.sp, [lambda e, xt=xt: e.dma_start(out=xt[:, :, :], in_=xview(X)[:, :, tt * TT:(tt + 1) * TT])],
              xtb, reads=[XB_[tt]], writes=[xtb])
        return xt, xtb

    def modulate(c, xt, xtb, li, sub, u=None, ub=None):
        u = c.u if u is None else u
        ub = c.ub if ub is None else ub
        for k in range(KC):
            sc = mcol(li, 3 * sub + 1, k)
            sh = mcol(li, 3 * sub + 0, k)
            if k % 2 == 0:
                S.op(S.dve, lambda e, k=k, sc=sc, sh=sh: e.tensor_scalar(out=u[:, k, :], in0=xt[:, k, :], scalar1=sc, scalar2=sh,
                                                                         op0=ALU.mult, op1=ALU.add),
                     reads=[xtb, modb], writes=[ub], partial=True)
            else:
                S.op(S.act, lambda e, k=k, sc=sc, sh=sh: e.activation(out=u[:, k, :], in_=xt[:, k, :], func=AF.Identity, bias=sh, scale=sc),
                     reads=[xtb, modb], writes=[ub], partial=True)

    def wslab(c, src, nk, ncols):
        t, tb = c.ws[c.wi % len(c.ws)]
        c.wi += 1
        view = t[:, 0:nk * ncols].rearrange("p (k c) -> p k c", k=nk)
        S.dma(S.pool, [lambda e, view=view, src=src: e.dma_start(out=view, in_=src)], tb, writes=[tb])
        return view, tb

    def alloc_ln(c):
        c.sq = [S.tile([128, TT], F32, f"sq{i}") for i in range(2)]
        c.mean, c.meanb = S.tile([128, TT], F32, "mean")
        c.msq, c.msqb = S.tile([128, TT], F32, "msq")
        c.rstd, c.rstdb = S.tile([128, TT], F32, "rstd")
        c.ytmp = [S.tile([128, TT], F32, f"ytmp{i}") for i in range(2)]
        c.ltmp = [S.tile([128, TT], F32, f"ltmp{i}") for i in range(3)]
        c.sqi = 0
        c.yi = 0
        c.li_ = 0

    def out_proj_ln(c, act, actb, kgroups, W, xt, xtb, li, sub, Xout, XoutB, tt):
        nk_total = sum(kgroups)
        Wv = W.rearrange("(k p) c -> p k c", p=128)
        gslot = 3 * sub + 2
        for cg in range(4):
            accs = [S.next_bank() for _ in range(4)]
            k0 = 0
            for gi, nk in enumerate(kgroups):
                wv, wb = wslab(c, Wv[:, k0:k0 + nk, cg * 512:(cg + 1) * 512], nk, 512)
                for m4 in range(4):
                    ps, psb = accs[m4]
                    rb = [wb] + (actb[k0:k0 + nk] if isinstance(actb, list) else [actb])

                    def mm(e, ps=ps, wv=wv, m4=m4, k0=k0, nk=nk):
                        for kk in range(nk):
                            ins = e.matmul(ps[:, :], lhsT=wv[:, kk, m4 * 128:(m4 + 1) * 128], rhs=act[:, k0 + kk, :],
                                           start=(k0 + kk == 0), stop=(k0 + kk == nk_total - 1))
                        return ins
                    S.op(S.pe, mm, reads=rb, writes=[psb], partial=(k0 > 0))
                k0 += nk
            for m4 in range(4):
                m = cg * 4 + m4
                ps, psb = accs[m4]
                yt, ytb = c.ytmp[c.yi % 2]
                c.yi += 1
                gcol = mcol(li, gslot, m)
                S.op(S.act, lambda e, ps=ps, yt=yt, gcol=gcol: e.activation(out=yt[:, :], in_=ps[:, :], func=AF.Identity, scale=gcol),
                     reads=[psb, modb], writes=[ytb])
                S.op(S.dve, lambda e, yt=yt, m=m: e.scalar_tensor_tensor(out=xt[:, m, :], in0=xt[:, m, :], scalar=ALPHA, in1=yt[:, :],
                                                                          op0=ALU.mult, op1=ALU.add),
                     reads=[ytb, xtb], writes=[xtb], partial=True)
        layer_norm(c, xt, xtb, li, sub)
        S.dma(S.sp, [lambda e: e.dma_start(out=xview(Xout)[:, :, tt * TT:(tt + 1) * TT], in_=xt[:, :, :])],
              xtb, reads=[xtb], writes=[XoutB[tt]])

    def layer_norm(c, xt, xtb, li, sub):
        s1, s1b = S.next_bank()
        s2, s2b = S.next_bank()

        def mm1(e):
            for k in range(KC):
                ins = e.matmul(s1[:, :], lhsT=ones_f[:, :], rhs=xt[:, k, :], start=(k == 0), stop=(k == KC - 1))
            return ins
        S.op(S.pe, mm1, reads=[xtb, ones_fb], writes=[s1b])
        for k in range(KC):
            sq, sqb = c.sq[c.sqi % 2]
            c.sqi += 1
            S.op(S.act, lambda e, sq=sq, k=k: e.activation(out=sq[:, :], in_=xt[:, k, :], func=AF.Square), reads=[xtb], writes=[sqb])
            S.op(S.pe, lambda e, sq=sq, k=k: e.matmul(s2[:, :], lhsT=ones_f[:, :], rhs=sq[:, :], start=(k == 0), stop=(k == KC - 1)),
                 reads=[sqb, ones_fb], writes=[s2b], partial=(k > 0))
        S.op(S.act, lambda e: e.activation(out=c.mean[:, :], in_=s1[:, :], func=AF.Copy, scale=1.0 / D), reads=[s1b], writes=[c.meanb])
        S.op(S.dve, lambda e: e.tensor_tensor(out=c.msq[:, :], in0=c.mean[:, :], in1=c.mean[:, :], op=ALU.mult), reads=[c.meanb], writes=[c.msqb])
        S.op(S.dve, lambda e: e.scalar_tensor_tensor(out=c.rstd[:, :], in0=s2[:, :], scalar=1.0 / D, in1=c.msq[:, :], op0=ALU.mult, op1=ALU.subtract),
             reads=[s2b, c.msqb], writes=[c.rstdb])
        S.op(S.dve, lambda e: e.tensor_scalar(out=c.rstd[:, :], in0=c.rstd[:, :], scalar1=LN_EPS, scalar2=None, op0=ALU.add),
             reads=[c.rstdb], writes=[c.rstdb])
        S.op(S.act, lambda e: e.activation(out=c.rstd[:, :], in_=c.rstd[:, :], func=AF.Sqrt), reads=[c.rstdb], writes=[c.rstdb])
        S.op(S.dve, lambda e: e.reciprocal(out=c.rstd[:, :], in_=c.rstd[:, :]), reads=[c.rstdb], writes=[c.rstdb])
        for k in range(KC):
            lt, ltb = c.ltmp[c.li_ % 3]
            c.li_ += 1
            S.op(S.pool, lambda e, lt=lt, k=k: e.tensor_tensor(out=lt[:, :], in0=xt[:, k, :], in1=c.mean[:, :], op=ALU.subtract),
                 reads=[xtb, c.meanb], writes=[ltb])
            S.op(S.dve, lambda e, lt=lt: e.tensor_tensor(out=lt[:, :], in0=lt[:, :], in1=c.rstd[:, :], op=ALU.mult),
                 reads=[ltb, c.rstdb], writes=[ltb])
            g = pcol(PV_LNG(li, sub), k)
            b = pcol(PV_LNB(li, sub), k)
            S.op(S.act, lambda e, lt=lt, k=k, g=g, b=b: e.activation(out=xt[:, k, :], in_=lt[:, :], func=AF.Identity, bias=b, scale=g),
                 reads=[ltb, pvb], writes=[xtb], partial=True)

    def phase_qkv(li, j, Xin, XinB_):
        S.cur = PERSIST
        c = alloc_common()
        stg = [S.tile([128, 4, TT], BF16, f"stg{i}") for i in range(2)]
        Wv = attn_w_qkv[j].rearrange("(k p) c -> p k c", p=128)
        Qv = QKV.rearrange("(c p) t -> p c t", p=128)
        nxt = load_x(c, Xin, XinB_, 0)
        si = 0
        for tt in range(NT):
            xt, xtb = nxt
            modulate(c, xt, xtb, li, 0)
            for sl in range(36):
                if sl == 2 and tt + 1 < NT:
                    nxt = load_x(c, Xin, XinB_, tt + 1)
                wv, wb = wslab(c, Wv[:, :, sl * 512:(sl + 1) * 512], KC, 512)
                st, stb = stg[si % 2]
                si += 1
                for c4 in range(4):
                    ps, psb = S.next_bank()

                    def mm(e, ps=ps, wv=wv, c4=c4):
                        for k in range(KC):
                            ins = e.matmul(ps[:, :], lhsT=wv[:, k, c4 * 128:(c4 + 1) * 128], rhs=c.u[:, k, :], start=(k == 0), stop=(k == KC - 1))
                        return ins
                    S.op(S.pe, mm, reads=[wb, c.ub], writes=[psb])
                    if c4 % 2 == 0:
                        S.op(S.act, lambda e, ps=ps, st=st, c4=c4: e.activation(out=st[:, c4, :], in_=ps[:, :], func=AF.Copy),
                             reads=[psb], writes=[stb], partial=True)
                    else:
                        S.op(S.dve, lambda e, ps=ps, st=st, c4=c4: e.tensor_copy(out=st[:, c4, :], in_=ps[:, :]),
                             reads=[psb], writes=[stb], partial=True)
                S.dma(S.sp, [lambda e, st=st, sl=sl, tt=tt: e.dma_start(out=Qv[:, sl * 4:(sl + 1) * 4, tt * TT:(tt + 1) * TT], in_=st[:, :, :])],
                      stb, reads=[stb], writes=[QKVB], partial=True)
        S.barrier()

    def phase_attn_core():
        S.cur = PERSIST
        qkvt = [S.tile([128, 3, SEQ], BF16, f"qkvt{i}") for i in range(2)]
        vtok = [S.tile([128, 32, 128], BF16, f"vtok{i}") for i in range(2)]
        OL = [S.tile([128, 2, SEQ], F32, f"OL{i}") for i in range(2)]
        oT = [S.tile([128, SEQ], BF16, f"oT{i}") for i in range(2)]
        bhg = [S.tile([128, 2, 128], F32, f"bhg{i}") for i in range(2)]
        ssb = [S.tile([128, 2, 128], F32, f"ssb{i}") for i in range(4)]
        pT = [S.tile([128, 2, 128], BF16, f"pT{i}") for i in range(4)]
        relb3 = relb[:, :].rearrange("p (c q) -> p c q", c=2)
        n_hg = 0
        n_blk = 0
        n_tr = 0
        for h in range(NH):
            ol, olb = OL[h % 2]
            slope = 2.0 ** (-(h + 1) / 2.0)
            for g in range(3):
                d = DILS[g]
                nb = SEQ // d // 128
                qt, qtb = qkvt[n_hg % 2]
                vt, vtb = vtok[n_hg % 2]
                bt, btb = bhg[n_hg % 2]
                n_hg += 1
                rows = [((g * 3 + s) * NH + h) * 128 for s in range(3)]
                S.dma(S.sp, [lambda e, qt=qt, s=s, r=rows[s]: e.dma_start(out=qt[:, s, :], in_=QKV[r:r + 128, :]) for s in range(3)],
                      qtb, reads=[QKVB], writes=[qtb])
                coef = -slope * d / SCALE
                S.op(S.pool, lambda e, bt=bt, coef=coef: e.tensor_scalar(out=bt[:, :, :], in0=relb3, scalar1=coef, scalar2=None, op0=ALU.mult),
                     reads=[relbb], writes=[btb])

                def tok(r, n, d=d):
                    st = n * 128 * d + r
                    return slice(st, st + 127 * d + 1, d)
                for b4 in range(8):
                    half = n_tr % 2
                    n_tr += 1

                    def trf(e, qt=qt, b4=b4, half=half, nb=nb, tok=tok):
                        for i in range(4):
                            bidx = b4 * 4 + i
                            r, n = bidx // nb, bidx % nb
                            ins = e.transpose(out=pst[half][:, i, :], in_=qt[:, 2, tok(r, n)], identity=ident[:, :])
                        return ins
                    S.op(S.pe, trf, reads=[qtb, identb], writes=[pstb[half]])
                    S.op(S.act, lambda e, vt=vt, b4=b4, half=half: e.activation(out=vt[:, b4 * 4:(b4 + 1) * 4, :], in_=pst[half][:, 0:4, :], func=AF.Copy),
                         reads=[pstb[half]], writes=[vtb], partial=True)
                SK = 2
                staged = {}

                def stage_a(bidx, qt=qt, qtb=qtb, bt=bt, btb=btb, nb=nb, tok=tok):
                    nonlocal n_blk
                    r, n = bidx // nb, bidx % nb
                    c0 = 1 if n == 0 else 0
                    ps, psb = S.next_bank()
                    ps3 = ps[:, 0:256].rearrange("p (c q) -> p c q", c=2)

                    def smm(e, ps3=ps3, qt=qt, r=r, n=n, c0=c0, tok=tok):
                        for cc in range(c0, 2):
                            ins = e.matmul(ps3[:, cc, :], lhsT=qt[:, 1, tok(r, n - 1 + cc)], rhs=qt[:, 0, tok(r, n)], start=True, stop=True)
                        return ins
                    S.op(S.pe, smm, reads=[qtb], writes=[psb])
                    sb_, sbb = ssb[n_blk % len(ssb)]
                    p_, pb = pT[n_blk % len(pT)]
                    n_blk += 1
                    S.op(S.dve, lambda e, sb_=sb_, ps3=ps3, bt=bt, c0=c0: e.tensor_tensor(out=sb_[:, c0:2, :], in0=ps3[:, c0:2, :], in1=bt[:, c0:2, :], op=ALU.add),
                         reads=[psb, btb], writes=[sbb])
                    S.op(S.act, lambda e, sb_=sb_, p_=p_, c0=c0: e.activation(out=p_[:, c0:2, :], in_=sb_[:, c0:2, :], func=AF.Exp, scale=SCALE),
                         reads=[sbb], writes=[pb])
                    staged[bidx] = (p_, pb, c0, r, n)

                def stage_b(bidx, vt=vt, vtb=vtb, ol=ol, olb=olb, g=g, tok=tok):
                    p_, pb, c0, r, n = staged.pop(bidx)
                    po, pob = S.next_bank()
                    po3 = po[:, 0:256].rearrange("p (c q) -> p c q", c=2)

                    def pvm(e, po3=po3, vt=vt, p_=p_, c0=c0, bidx=bidx):
                        for cc in range(c0, 2):
                            e.matmul(po3[:, 0, :], lhsT=vt[:, bidx - 1 + cc, :], rhs=p_[:, cc, :], start=(cc == c0), stop=(cc == 1))
                        for cc in range(c0, 2):
                            ins = e.matmul(po3[:, 1, :], lhsT=ones_b[:, :], rhs=p_[:, cc, :], start=(cc == c0), stop=(cc == 1))
                        return ins
                    S.op(S.pe, pvm, reads=[vtb, pb, ones_bb], writes=[pob])
                    dst = ol[:, :, tok(r, n)]
                    if g == 0:
                        S.op(S.act, lambda e, dst=dst, po3=po3: e.activation(out=dst, in_=po3, func=AF.Copy), reads=[pob], writes=[olb], partial=True)
                    else:
                        S.op(S.dve, lambda e, dst=dst, po3=po3: e.tensor_tensor(out=dst, in0=dst, in1=po3, op=ALU.add),
                             reads=[pob, olb], writes=[olb], partial=True)

                for i in range(32 + SK):
                    if i < 32:
                        stage_a(i)
                    if i >= SK:
                        stage_b(i - SK)
            o_, ob = oT[h % 2]
            S.op(S.dve, lambda e, ol=ol: e.reciprocal(out=ol[:, 1, :], in_=ol[:, 1, :]), reads=[olb], writes=[olb], partial=True)
            S.op(S.dve, lambda e, ol=ol, o_=o_: e.tensor_tensor(out=o_[:, :], in0=ol[:, 0, :], in1=ol[:, 1, :], op=ALU.mult), reads=[olb], writes=[ob])
            S.dma(S.sp, [lambda e, o_=o_, h=h: e.dma_start(out=OT[h * 128:(h + 1) * 128, :], in_=o_[:, :])], ob, reads=[ob], writes=[OTB], partial=True)
        S.barrier()

    def phase_attn_out(li, j, Xin, XinB_, Xout, XoutB):
        S.cur = PERSIST
        c = alloc_common(n_x=2)
        alloc_ln(c)
        ot = [S.tile([128, KC, TT], BF16, f"ot{i}") for i in range(2)]
        OTv = OT.rearrange("(k p) t -> p k t", p=128)
        def loads(tt):
            xt, xtb = load_x(c, Xin, XinB_, tt)
            o_, ob = ot[tt % 2]
            S.dma(S.sp, [lambda e, o_=o_, tt=tt: e.dma_start(out=o_[:, :, :], in_=OTv[:, :, tt * TT:(tt + 1) * TT])], ob, reads=[OTB], writes=[ob])
            return xt, xtb, o_, ob
        nxt = loads(0)
        for tt in range(NT):
            xt, xtb, o_, ob = nxt
            if tt + 1 < NT:
                nxt = loads(tt + 1)
            out_proj_ln(c, o_, ob, [KC], attn_w_o[j], xt, xtb, li, 0, Xout, XoutB, tt)
        S.barrier()

    def phase_ffn(li, j, Xin, XinB_, Xout, XoutB):
        S.cur = PERSIST
        c = alloc_common(n_x=2)
        alloc_ln(c)
        h, hb_ = S.tile([128, JC, TT], BF16, "h")
        hb = [S.buf() for _ in range(JC)]
        sg = [S.tile([128, TT], BF16, f"sg{i}") for i in range(5)]
        Wv = ffn_w_in[j].rearrange("(k p) c -> p k c", p=128)
        ns = 0
        nxt = load_x(c, Xin, XinB_, 0)
        modulate(c, nxt[0], nxt[1], li, 1)
        for tt in range(NT):
            xt, xtb = nxt
            for jg in range(11):
                wg, wgb = wslab(c, Wv[:, :, jg * 512:(jg + 1) * 512], KC, 512)
                wu, wub = wslab(c, Wv[:, :, DFF + jg * 512:DFF + (jg + 1) * 512], KC, 512)
                sgs = []
                for j4 in range(4):
                    pg, pgb = S.next_bank()

                    def mmg(e, pg=pg, wg=wg, j4=j4):
                        for k in range(KC):
                            ins = e.matmul(pg[:, :], lhsT=wg[:, k, j4 * 128:(j4 + 1) * 128], rhs=c.u[:, k, :], start=(k == 0), stop=(k == KC - 1))
                        return ins
                    S.op(S.pe, mmg, reads=[wgb, c.ub], writes=[pgb])
                    s_, sb_ = sg[ns % len(sg)]
                    ns += 1
                    S.op(S.act, lambda e, s_=s_, pg=pg: e.activation(out=s_[:, :], in_=pg[:, :], func=AF.Silu), reads=[pgb], writes=[sb_])
                    sgs.append((s_, sb_))
                for j4 in range(4):
                    jj = jg * 4 + j4
                    pu, pub = S.next_bank()

                    def mmu(e, pu=pu, wu=wu, j4=j4):
                        for k in range(KC):
                            ins = e.matmul(pu[:, :], lhsT=wu[:, k, j4 * 128:(j4 + 1) * 128], rhs=c.u[:, k, :], start=(k == 0), stop=(k == KC - 1))
                        return ins
                    S.op(S.pe, mmu, reads=[wub, c.ub], writes=[pub])
                    s_, sb_ = sgs[j4]
                    S.op(S.dve, lambda e, s_=s_, pu=pu, jj=jj: e.tensor_tensor(out=h[:, jj, :], in0=s_[:, :], in1=pu[:, :], op=ALU.mult),
                         reads=[sb_, pub], writes=[hb[jj]])
            if tt + 1 < NT:
                nxt = load_x(c, Xin, XinB_, tt + 1)
                modulate(c, nxt[0], nxt[1], li, 1)
            out_proj_ln(c, h, hb, [11, 11, 11, 11], ffn_w_out[j], xt, xtb, li, 1, Xout, XoutB, tt)
        S.barrier()

    def phase_rglru(li, j, Xin, XinB_, Xout, XoutB):
        S.cur = PERSIST
        c = alloc_common(n_x=1, n_w=2)
        alloc_ln(c)
        gb, gbb_ = S.tile([128, KC, TT], BF16, "gb")
        gbb = [S.buf() for _ in range(KC)]
        y, yb_ = S.tile([128, KC, TT], BF16, "y")
        yb = [S.buf() for _ in range(KC)]
        rec = [S.tile([128, TT + 3], F32, f"rec{i}") for i in range(4)]
        reh, rehb = S.tile([128, KC, 3], F32, "reh")
        hc, hcb = S.tile([128, KC], F32, "hc")
        xc = [S.tile([128, 2, TT], F32, f"xc{i}") for i in range(2)]
        xcb16 = [S.tile([128, 2, TT], BF16, f"xcb{i}") for i in range(2)]
        GA, GAb = S.tile([128, 8, 2, 256], BF16, "GA")
        GX, GXb = S.tile([128, 8, 2, 256], BF16, "GX")
        tmps = [{nm: S.tile([128, TT], F32, f"{nm}{q}") for nm in ("r", "i", "a", "a2", "bx", "hh")} for q in range(2)]
        ntmp = 0
        S.dma(S.pool, [lambda e, n=n: e.dma_start(out=GA[:, n, :, :], in_=rg_ga_w[j, n].rearrange("(ic p) o -> p ic o", p=128)) for n in range(8)], GAb, writes=[GAb])
        S.dma(S.pool, [lambda e, n=n: e.dma_start(out=GX[:, n, :, :], in_=rg_gx_w[j, n].rearrange("(ic p) o -> p ic o", p=128)) for n in range(8)], GXb, writes=[GXb])
        S.op(S.dve, lambda e: e.memset(reh[:, :, :], 0.0), writes=[rehb])
        S.op(S.dve, lambda e: e.memset(hc[:, :], 0.0), writes=[hcb])
        Wv = rg_w_in[j].rearrange("(k p) c -> p k c", p=128)
        nrec = 0
        nxc = 0
        for tt in range(NT):
            xt, xtb = load_x(c, Xin, XinB_, tt)
            modulate(c, xt, xtb, li, 0)
            for sl in range(4):
                wv, wb = wslab(c, Wv[:, :, sl * 512:(sl + 1) * 512], KC, 512)
                for c4 in range(4):
                    m = sl * 4 + c4
                    ps, psb = S.next_bank()

                    def mm(e, ps=ps, wv=wv, c4=c4):
                        for k in range(KC):
                            ins = e.matmul(ps[:, :], lhsT=wv[:, k, c4 * 128:(c4 + 1) * 128], rhs=c.u[:, k, :], start=(k == 0), stop=(k == KC - 1))
                        return ins
                    S.op(S.pe, mm, reads=[wb, c.ub], writes=[psb])
                    S.op(S.act, lambda e, ps=ps, m=m: e.activation(out=gb[:, m, :], in_=ps[:, :], func=AF.Gelu_apprx_tanh), reads=[psb], writes=[gbb[m]])
            for sl in range(4):
                wv, wb = wslab(c, Wv[:, :, D + sl * 512:D + (sl + 1) * 512], KC, 512)
                for pr in range(2):
                    xc_, xcb_ = xc[nxc % 2]
                    x16, x16b = xcb16[nxc % 2]
                    nxc += 1
                    nbk = sl * 2 + pr
                    for ic in range(2):
                        m = nbk * 2 + ic
                        c4 = pr * 2 + ic
                        ps, psb = S.next_bank()

                        def mm(e, ps=ps, wv=wv, c4=c4):
                            for k in range(KC):
                                ins = e.matmul(ps[:, :], lhsT=wv[:, k, c4 * 128:(c4 + 1) * 128], rhs=c.u[:, k, :], start=(k == 0), stop=(k == KC - 1))
                            return ins
                        S.op(S.pe, mm, reads=[wb, c.ub], writes=[psb])
                        rc, rcb = rec[nrec % 4]
                        nrec += 1
                        S.op(S.act, lambda e, rc=rc, ps=ps: e.activation(out=rc[:, 3:TT + 3], in_=ps[:, :], func=AF.Copy), reads=[psb], writes=[rcb], partial=True)
                        S.op(S.pool, lambda e, rc=rc, m=m: e.tensor_copy(out=rc[:, 0:3], in_=reh[:, m, :]), reads=[rehb], writes=[rcb], partial=True)
                        S.op(S.act, lambda e, rc=rc, xc_=xc_, ic=ic, m=m: e.activation(out=xc_[:, ic, :], in_=rc[:, 0:TT], func=AF.Identity,
                                                                                     bias=pcol(PV_CB(j), m), scale=pcol(PV_CW(j, 0), m)),
                             reads=[rcb, pvb], writes=[xcb_], partial=True)
                        for w in range(1, 4):
                            eng = S.dve
                            S.op(eng, lambda e, rc=rc, xc_=xc_, ic=ic, m=m, w=w: e.scalar_tensor_tensor(
                                out=xc_[:, ic, :], in0=rc[:, w:w + TT], scalar=pcol(PV_CW(j, w), m), in1=xc_[:, ic, :], op0=ALU.mult, op1=ALU.add),
                                reads=[rcb, pvb, xcb_], writes=[xcb_], partial=True)
                        S.op(S.pool, lambda e, rc=rc, m=m: e.tensor_copy(out=reh[:, m, :], in_=rc[:, TT:TT + 3]), reads=[rcb], writes=[rehb], partial=True)
                    S.op(S.act, lambda e, xc_=xc_, x16=x16: e.activation(out=x16[:, :, :], in_=xc_[:, :, :], func=AF.Copy), reads=[xcb_], writes=[x16b])
                    for oc in range(2):
                        m = nbk * 2 + oc
                        pa, pab = S.next_bank()
                        px, pxb = S.next_bank()

                        def mma(e, pa=pa, x16=x16, nbk=nbk, oc=oc):
                            for ic in range(2):
                                ins = e.matmul(pa[:, :], lhsT=GA[:, nbk, ic, oc * 128:(oc + 1) * 128], rhs=x16[:, ic, :], start=(ic == 0), stop=(ic == 1))
                            return ins

                        def mmx(e, px=px, x16=x16, nbk=nbk, oc=oc):
                            for ic in range(2):
                                ins = e.matmul(px[:, :], lhsT=GX[:, nbk, ic, oc * 128:(oc + 1) * 128], rhs=x16[:, ic, :], start=(ic == 0), stop=(ic == 1))
                            return ins
                        S.op(S.pe, mma, reads=[GAb, x16b], writes=[pab])
                        S.op(S.pe, mmx, reads=[GXb, x16b], writes=[pxb])
                        tmp = tmps[ntmp % 2]
                        ntmp += 1
                        r_, rb_ = tmp["r"]
                        i_, ib_ = tmp["i"]
                        a_, ab_ = tmp["a"]
                        a2, a2b = tmp["a2"]
                        bx, bxb = tmp["bx"]
                        hh, hhb = tmp["hh"]
                        S.op(S.act, lambda e, pa=pa, r_=r_, m=m: e.activation(out=r_[:, :], in_=pa[:, :], func=AF.Sigmoid, bias=pcol(PV_GAB(j), m)),
                             reads=[pab, pvb], writes=[rb_])
                        S.op(S.act, lambda e, px=px, i_=i_, m=m: e.activation(out=i_[:, :], in_=px[:, :], func=AF.Sigmoid, bias=pcol(PV_GXB(j), m)),
                             reads=[pxb, pvb], writes=[ib_])
                        cscol = cs[:, j * KC + m:j * KC + m + 1]
                        S.op(S.act, lambda e, a_=a_, r_=r_, cscol=cscol: e.activation(out=a_[:, :], in_=r_[:, :], func=AF.Exp, scale=cscol),
                             reads=[rb_, csb], writes=[ab_])
                        S.op(S.pool, lambda e, a2=a2, a_=a_: e.tensor_tensor(out=a2[:, :], in0=a_[:, :], in1=a_[:, :], op=ALU.mult), reads=[ab_], writes=[a2b])
                        S.op(S.dve, lambda e, a2=a2: e.tensor_scalar(out=a2[:, :], in0=a2[:, :], scalar1=-1.0, scalar2=1.0, op0=ALU.mult, op1=ALU.add),
                             reads=[a2b], writes=[a2b])
                        S.op(S.act, lambda e, a2=a2: e.activation(out=a2[:, :], in_=a2[:, :], func=AF.Sqrt), reads=[a2b], writes=[a2b])
                        S.op(S.pool, lambda e, bx=bx, i_=i_, xc_=xc_, oc=oc: e.tensor_tensor(out=bx[:, :], in0=i_[:, :], in1=xc_[:, oc, :], op=ALU.mult),
                             reads=[ib_, xcb_], writes=[bxb])
                        S.op(S.dve, lambda e, bx=bx, a2=a2: e.tensor_tensor(out=bx[:, :], in0=bx[:, :], in1=a2[:, :], op=ALU.mult), reads=[bxb, a2b], writes=[bxb])
                        S.op(S.dve, lambda e, hh=hh, a_=a_, bx=bx, m=m: e.tensor_tensor_scan(out=hh[:, :], data0=a_[:, :], data1=bx[:, :], initial=hc[:, m:m + 1],
                                                                                            op0=ALU.mult, op1=ALU.add),
                             reads=[ab_, bxb, hcb], writes=[hhb])
                        S.op(S.dve, lambda e, hh=hh, m=m: e.tensor_copy(out=hc[:, m:m + 1], in_=hh[:, TT - 1:TT]), reads=[hhb], writes=[hcb], partial=True)
                        S.op(S.dve, lambda e, hh=hh, m=m: e.tensor_tensor(out=y[:, m, :], in0=gb[:, m, :], in1=hh[:, :], op=ALU.mult),
                             reads=[hhb, gbb[m]], writes=[yb[m]])
            out_proj_ln(c, y, yb, [KC], rg_w_out[j], xt, xtb, li, 0, Xout, XoutB, tt)
        S.barrier()

    def phase_moe(li, j, Xin, XinB_, Xout, XoutB):
        S.cur = PERSIST
        c = alloc_common(n_x=1, n_w=3)
        alloc_ln(c)
        u32 = [S.tile([128, TT], F32, f"u32_{i}") for i in range(2)]
        h, hb_ = S.tile([128, JC, TT], BF16, "h")
        hb = [S.buf() for _ in range(JC)]
        sg = [S.tile([128, TT], BF16, f"sg{i}") for i in range(5)]
        wr, wrb = S.tile([128, KC, NE], F32, "wr")
        lg, lgb = S.tile([128, 4, NE], F32, "lg")
        mx, mxb = S.tile([128, 4, 8], F32, "mx")
        wtk, wtkb = S.tile([128, 4, NE], F32, "wtk")
        den, denb = S.tile([128, 4, 1], F32, "den")
        wT, wTb = S.tile([NE, TT], F32, "wT")
        sel, selb = S.tile([NE, NE, 128], F32, "sel")
        wB, wBb_ = S.tile([128, NE, TT], F32, "wB")
        wBb = [S.buf() for _ in range(NE)]
        S.dma(S.sp, [lambda e: e.dma_start(out=wr[:, :, :], in_=moe_w_router[j].rearrange("(k p) n -> p k n", p=128))], wrb, writes=[wrb])
        S.dma(S.sp, [lambda e: e.dma_start(out=sel[:, :, :], in_=sel_in.rearrange("k (e m) -> k e m", e=NE))], selb, writes=[selb])
        ns = 0
        for tt in range(NT):
            xt, xtb = load_x(c, Xin, XinB_, tt)
            pls = [S.next_bank() for _ in range(4)]
            for k in range(KC):
                sc = mcol(li, 4, k)
                sh = mcol(li, 3, k)
                uk, ukb = u32[k % 2]
                S.op(S.dve, lambda e, k=k, sc=sc, sh=sh, uk=uk: e.tensor_scalar(out=uk[:, :], in0=xt[:, k, :], scalar1=sc, scalar2=sh, op0=ALU.mult, op1=ALU.add),
                     reads=[xtb, modb], writes=[ukb])
                S.op(S.act, lambda e, k=k, uk=uk: e.activation(out=c.u[:, k, :], in_=uk[:, :], func=AF.Copy), reads=[ukb], writes=[c.ub], partial=True)
                for s4 in range(4):
                    pl, plb = pls[s4]
                    S.op(S.pe, lambda e, pl=pl, uk=uk, k=k, s4=s4: e.matmul(pl[:, 0:NE], lhsT=uk[:, s4 * 128:(s4 + 1) * 128], rhs=wr[:, k, :], start=(k == 0), stop=(k == KC - 1)),
                         reads=[ukb, wrb], writes=[plb], partial=(k > 0))
            for k in range(KC):
                S.op(S.pool, lambda e, k=k: e.tensor_scalar(out=xt[:, k, :], in0=xt[:, k, :], scalar1=ALPHA, scalar2=None, op0=ALU.mult),
                     reads=[xtb], writes=[xtb], partial=True)
            for s4 in range(4):
                pl, plb = pls[s4]
                S.op(S.dve, lambda e, pl=pl, s4=s4: e.tensor_copy(out=lg[:, s4, :], in_=pl[:, 0:NE]), reads=[plb], writes=[lgb], partial=True)
            for s4 in range(4):
                S.op(S.dve, lambda e, s4=s4: e.max(out=mx[:, s4, :], in_=lg[:, s4, :]), reads=[lgb], writes=[mxb], partial=True)
            for s4 in range(4):
                S.op(S.dve, lambda e, s4=s4: e.tensor_scalar(out=wtk[:, s4, :], in0=lg[:, s4, :], scalar1=mx[:, s4, 0:1], scalar2=None, op0=ALU.subtract),
                     reads=[lgb, mxb], writes=[wtkb], partial=True)
            S.op(S.act, lambda e: e.activation(out=wtk[:, :, :], in_=wtk[:, :, :], func=AF.Exp), reads=[wtkb], writes=[wtkb])
            for s4 in range(4):
                S.op(S.dve, lambda e, s4=s4: e.scalar_tensor_tensor(out=wtk[:, s4, :], in0=lg[:, s4, :], scalar=mx[:, s4, 1:2], in1=wtk[:, s4, :],
                                                                    op0=ALU.is_ge, op1=ALU.mult),
                     reads=[lgb, mxb, wtkb], writes=[wtkb], partial=True)
            S.op(S.dve, lambda e: e.tensor_tensor(out=den[:, :, :], in0=mx[:, :, 1:2], in1=mx[:, :, 0:1], op=ALU.subtract), reads=[mxb], writes=[denb])
            S.op(S.act, lambda e: e.activation(out=den[:, :, :], in_=den[:, :, :], func=AF.Exp), reads=[denb], writes=[denb])
            S.op(S.dve, lambda e: e.tensor_scalar(out=den[:, :, :], in0=den[:, :, :], scalar1=1.0, scalar2=None, op0=ALU.add), reads=[denb], writes=[denb])
            S.op(S.dve, lambda e: e.reciprocal(out=den[:, :, :], in_=den[:, :, :]), reads=[denb], writes=[denb])
            for s4 in range(4):
                S.op(S.dve, lambda e, s4=s4: e.tensor_scalar(out=wtk[:, s4, :], in0=wtk[:, s4, :], scalar1=den[:, s4, 0:1], scalar2=None, op0=ALU.mult),
                     reads=[denb, wtkb], writes=[wtkb], partial=True)
            pt, ptb = S.next_bank()

            def trw(e, pt=pt):
                for s4 in range(4):
                    ins = e.transpose(out=pt[0:NE, s4 * 128:(s4 + 1) * 128], in_=wtk[:, s4, :], identity=identf[:, :])
                return ins
            S.op(S.pe, trw, reads=[wtkb, identfb], writes=[ptb])
            S.op(S.dve, lambda e, pt=pt: e.tensor_copy(out=wT[:, :], in_=pt[0:NE, :]), reads=[ptb], writes=[wTb])
            for ex in range(NE):
                pb_, pbb = S.next_bank()
                S.op(S.pe, lambda e, pb_=pb_, ex=ex: e.matmul(pb_[:, :], lhsT=sel[:, ex, :], rhs=wT[:, :], start=True, stop=True), reads=[selb, wTb], writes=[pbb])
                S.op(S.act, lambda e, pb_=pb_, ex=ex: e.activation(out=wB[:, ex, :], in_=pb_[:, :], func=AF.Copy), reads=[pbb], writes=[wBb[ex]])
            for ex in range(NE):
                Wv = moe_w_in[j, ex].rearrange("(k p) c -> p k c", p=128)
                for jg in range(11):
                    wg, wgb = wslab(c, Wv[:, :, jg * 512:(jg + 1) * 512], KC, 512)
                    wu, wub = wslab(c, Wv[:, :, DFF + jg * 512:DFF + (jg + 1) * 512], KC, 512)
                    sgs = []
                    for j4 in range(4):
                        pg, pgb = S.next_bank()

                        def mmg(e, pg=pg, wg=wg, j4=j4):
                            for k in range(KC):
                                ins = e.matmul(pg[:, :], lhsT=wg[:, k, j4 * 128:(j4 + 1) * 128], rhs=c.u[:, k, :], start=(k == 0), stop=(k == KC - 1))
                            return ins
                        S.op(S.pe, mmg, reads=[wgb, c.ub], writes=[pgb])
                        s_, sb_ = sg[ns % len(sg)]
                        ns += 1
                        S.op(S.act, lambda e, s_=s_, pg=pg: e.activation(out=s_[:, :], in_=pg[:, :], func=AF.Silu), reads=[pgb], writes=[sb_])
                        sgs.append((s_, sb_))
                    for j4 in range(4):
                        jj = jg * 4 + j4
                        pu, pub = S.next_bank()

                        def mmu(e, pu=pu, wu=wu, j4=j4):
                            for k in range(KC):
                                ins = e.matmul(pu[:, :], lhsT=wu[:, k, j4 * 128:(j4 + 1) * 128], rhs=c.u[:, k, :], start=(k == 0), stop=(k == KC - 1))
                            return ins
                        S.op(S.pe, mmu, reads=[wub, c.ub], writes=[pub])
                        s_, sb_ = sgs[j4]
                        S.op(S.dve, lambda e, s_=s_, pu=pu, jj=jj: e.tensor_tensor(out=h[:, jj, :], in0=s_[:, :], in1=pu[:, :], op=ALU.mult),
                             reads=[sb_, pub], writes=[hb[jj]])
                Wo = moe_w_out[j, ex].rearrange("(k p) c -> p k c", p=128)
                for cg in range(4):
                    accs = [S.next_bank() for _ in range(4)]
                    for gi in range(4):
                        wv, wb = wslab(c, Wo[:, gi * 11:(gi + 1) * 11, cg * 512:(cg + 1) * 512], 11, 512)
                        for m4 in range(4):
                            ps, psb = accs[m4]

                            def mm(e, ps=ps, wv=wv, m4=m4, gi=gi):
                                for kk in range(11):
                                    ins = e.matmul(ps[:, :], lhsT=wv[:, kk, m4 * 128:(m4 + 1) * 128], rhs=h[:, gi * 11 + kk, :],
                                                   start=(gi == 0 and kk == 0), stop=(gi == 3 and kk == 10))
                                return ins
                            S.op(S.pe, mm, reads=[wb] + hb[gi * 11:(gi + 1) * 11], writes=[psb], partial=(gi > 0))
                    for m4 in range(4):
                        m = cg * 4 + m4
                        ps, psb = accs[m4]
                        yt, ytb = c.ytmp[c.yi % 2]
                        c.yi += 1
                        S.op(S.dve, lambda e, ps=ps, yt=yt, ex=ex: e.tensor_tensor(out=yt[:, :], in0=ps[:, :], in1=wB[:, ex, :], op=ALU.mult),
                             reads=[psb, wBb[ex]], writes=[ytb])
                        S.op(S.dve, lambda e, yt=yt, m=m: e.scalar_tensor_tensor(out=xt[:, m, :], in0=yt[:, :], scalar=mcol(li, 5, m), in1=xt[:, m, :],
                                                                                  op0=ALU.mult, op1=ALU.add),
                             reads=[ytb, xtb, modb], writes=[xtb], partial=True)
            layer_norm(c, xt, xtb, li, 1)
            S.dma(S.sp, [lambda e, xt=xt, tt=tt: e.dma_start(out=xview(Xout)[:, :, tt * TT:(tt + 1) * TT], in_=xt[:, :, :])],
                  xtb, reads=[xtb], writes=[XoutB[tt]])
        S.barrier()

    phase_ada()
    if dbg == "ada":
        S.dma(S.sp, [lambda e: e.dma_start(out=outT[0:128, 0:DEPTH * 96], in_=mod[:, :])], modb, reads=[modb])
        S.dma(S.sp, [lambda e: e.dma_start(out=outT[128:256, 0:2 * KC], in_=cs[:, :])], csb, reads=[csb])
        S.barrier()
        S.emit()
        S.declared = declared
        return nc, S
    steps = []
    for li in range(nlayers):
        steps.append((li, 0))
        steps.append((li, 1))
    if stop is not None:
        steps = [s for s in steps if s <= stop]
    cur, curB = xT_in, XinB
    for si, (li, sub) in enumerate(steps):
        last = si == len(steps) - 1
        if last:
            nxt, nxtB = outT, outB
        elif cur is XA:
            nxt, nxtB = XB, XBB
        else:
            nxt, nxtB = XA, XAB
        j = li // 2
        if sub == 0 and li % 2 == 0:
            phase_qkv(li, j, cur, curB)
            if dbg in ("qkv", "core"):
                if dbg == "core":
                    phase_attn_core()
                S.cur = PERSIST
                tb16, tb16b = S.tile([128, SEQ], BF16, "dbg16")
                tb32, tb32b = S.tile([128, SEQ], F32, "dbg32")
                rows = [0, 2048, 4096] if dbg == "qkv" else [0, 128, 1920]
                for ri, r in enumerate(rows):
                    src = QKV if dbg == "qkv" else OT
                    S.dma(S.sp, [lambda e, r=r, src=src: e.dma_start(out=tb16[:, :], in_=src[r:r + 128, :])], tb16b, reads=[QKVB, OTB], writes=[tb16b])
                    S.op(S.act, lambda e: e.activation(out=tb32[:, :], in_=tb16[:, :], func=AF.Copy), reads=[tb16b], writes=[tb32b])
                    S.dma(S.sp, [lambda e, ri=ri: e.dma_start(out=outT[ri * 128:(ri + 1) * 128, :], in_=tb32[:, :])], tb32b, reads=[tb32b])
                S.barrier()
                S.emit()
                S.declared = declared
                return nc, S
            phase_attn_core()
            phase_attn_out(li, j, cur, curB, nxt, nxtB)
        elif sub == 0:
            phase_rglru(li, j, cur, curB, nxt, nxtB)
        elif li % 2 == 0:
            phase_ffn(li, j, cur, curB, nxt, nxtB)
        else:
            phase_moe(li, j, cur, curB, nxt, nxtB)
        cur, curB = nxt, nxtB
    S.barrier()
    S.emit()
    S.declared = declared
    return nc, S


def host_consts():
    qi = np.arange(128)[None, :]
    kj = np.arange(128)[:, None]
    prev = np.where(kj >= qi, (qi + 128 - kj).astype(np.float32), BIGREL)
    curr = np.where(kj <= qi, (qi - kj).astype(np.float32), BIGREL)
    relb = np.concatenate([prev, curr], axis=1).astype(np.float32)
    ident = np.eye(128, dtype=np.float32)
    sel = np.zeros((NE, NE, 128), np.float32)
    for e_ in range(NE):
        sel[e_, e_, :] = 1.0
    return relb, ident, sel.reshape(NE, NE * 128)


def col_layout(v):
    return np.ascontiguousarray(np.asarray(v, np.float32).reshape(KC, 128).T)


def make_in_maps(inputs, cores, declared=None):
    f = lambda k: np.ascontiguousarray(np.asarray(inputs[k], dtype=np.float32))
    relb, ident, sel = host_consts()
    pv = np.zeros((128, NPV, KC), np.float32)
    for i in range(DEPTH):
        for s in range(2):
            pv[:, PV_LNG(i, s)] = col_layout(inputs["ln_g"][i, s])
            pv[:, PV_LNB(i, s)] = col_layout(inputs["ln_b"][i, s])
    for j in range(2):
        for w in range(4):
            pv[:, PV_CW(j, w)] = col_layout(inputs["rg_conv_w"][j, w])
        pv[:, PV_CB(j)] = col_layout(inputs["rg_conv_b"][j])
        pv[:, PV_GAB(j)] = col_layout(inputs["rg_gate_a_b"][j])
        pv[:, PV_GXB(j)] = col_layout(inputs["rg_gate_x_b"][j])
        pv[:, PV_LAM(j)] = col_layout(inputs["rg_lambda"][j])
    pv = np.ascontiguousarray(pv.reshape(128, NPV * KC))
    shared = {k: f(k) for k in (kk for kk in ("ada_w", "ada_b", "attn_w_qkv", "attn_w_o", "rg_w_in", "rg_gate_a_w", "rg_gate_x_w", "rg_w_out",
                                "ffn_w_in", "ffn_w_out", "moe_w_router", "moe_w_in", "moe_w_out") if declared is None or kk in declared)}
    x = np.asarray(inputs["x"], np.float32)
    cc = np.asarray(inputs["c"], np.float32)
    maps = []
    for b in cores:
        m = dict(shared)
        m["xT"] = np.ascontiguousarray(x[b].T)
        m["cvec"] = col_layout(cc[b])
        m["pvec"] = pv
        m["relb"] = relb
        m["ident"] = ident
        m["sel"] = sel
        if declared is not None:
            m = {k: v for k, v in m.items() if k in declared}
        maps.append(m)
    return maps


_CACHE = {}


def kernel(**inputs):
    if "nc" not in _CACHE:
        _CACHE["nc"] = build_program()
    nc, S = _CACHE["nc"]
    maps = make_in_maps(inputs, list(range(8)))
    res = run_bass_kernel_spmd(nc, maps, core_ids=list(range(8)))
    out = np.stack([np.ascontiguousarray(r["outT"].T) for r in res.results], axis=0)
    return out.astype(np.float32)
```
